# Optimizing a Trainium2 kernel written in Bass

```python
import math
import jax, jax.numpy as jnp
from jax import lax
import numpy as np

D_MODEL = 1024
BATCH = 2
SEQ = 8192
DEPTH = 1

GRID_W = 64
CTX_LEN = 256
N_DA_HEADS = 4
DA_HEAD_DIM = 64
DA_V_DIM = 2 * DA_HEAD_DIM
QK_WIDTH = N_DA_HEADS * 2 * DA_HEAD_DIM
ATTN_WIDTH = N_DA_HEADS * DA_V_DIM
N_FOURIER_GROUPS = 4
FOURIER_GROUP_DIM = 128
FOURIER_WIDTH = N_FOURIER_GROUPS * FOURIER_GROUP_DIM
MIX_WIDTH = ATTN_WIDTH + FOURIER_WIDTH
IN_PROJ_WIDTH = 2 * QK_WIDTH + ATTN_WIDTH + FOURIER_WIDTH
ROPE_THETA = 10000.0
ROPE_AXIS_DIM = DA_HEAD_DIM // 2
Q_BLOCK = 128
N_GROUPS = 4
EXPERTS_PER_GROUP = 8
N_EXPERTS = N_GROUPS * EXPERTS_PER_GROUP
TOP_K = 2
D_EXPERT = 512
MOE_BLOCK = 128
N_MOD = 6
EPS = 1e-6

kernel_name = 'hybrid_diffattn_fnet_hmoe_dit_block'


def rms_norm(x, g):
    xf = x.astype(jnp.float32)
    y = xf * lax.rsqrt(jnp.mean(xf * xf, axis=-1, keepdims=True) + EPS)
    return (y * g.astype(jnp.float32)).astype(x.dtype)


def modulate(h, shift, scale):
    return h * (1 + scale) + shift


def axial_rope_tables(rows, dtype):
    r, col = jnp.meshgrid(jnp.arange(rows), jnp.arange(GRID_W), indexing='ij')
    pos = jnp.stack([r.reshape(-1), col.reshape(-1)], axis=-1).astype(jnp.float32)
    inv_freq = ROPE_THETA ** (-jnp.arange(0, ROPE_AXIS_DIM, 2, dtype=jnp.float32) / ROPE_AXIS_DIM)
    ang = pos[:, :, None] * inv_freq
    ang = jnp.concatenate([ang, ang], axis=-1)
    return jnp.cos(ang).astype(dtype), jnp.sin(ang).astype(dtype)


def apply_axial_rope(x, cos, sin):
    xr = x.reshape(*x.shape[:-1], 2, ROPE_AXIS_DIM)
    x1, x2 = jnp.split(xr, 2, axis=-1)
    rot = jnp.concatenate([-x2, x1], axis=-1)
    c = cos[None, :, None, None]
    s = sin[None, :, None, None]
    return (xr * c + rot * s).reshape(x.shape)


def diff_lambda(lq1, lk1, lq2, lk2, lambda_init):
    f = lambda a: a.astype(jnp.float32)
    return jnp.exp(jnp.sum(f(lq1) * f(lk1))) - jnp.exp(jnp.sum(f(lq2) * f(lk2))) + lambda_init


def diff_attention(q, k, v, lam, g_subln, lambda_init):
    b, n = q.shape[:2]
    nb = n // Q_BLOCK
    qb = jnp.moveaxis(q.reshape(b, nb, Q_BLOCK, *q.shape[2:]), 1, 0)
    scale = DA_HEAD_DIM ** -0.5

    def block(qi):
        s = jnp.einsum('bqhmd,bkhmd->bhmqk', qi, k, preferred_element_type=jnp.float32) * scale
        p = jax.nn.softmax(s, axis=-1)
        a = p[:, :, 0] - lam * p[:, :, 1]
        return jnp.einsum('bhqk,bkhv->bqhv', a.astype(v.dtype), v)

    o = lax.map(block, qb)
    o = jnp.moveaxis(o, 0, 1).reshape(b, n, N_DA_HEADS, DA_V_DIM)
    o = rms_norm(o, g_subln) * (1.0 - lambda_init)
    return o.reshape(b, n, ATTN_WIDTH)


def fourier_mix(f, w_fourier):
    b, n, _ = f.shape
    fg = f.reshape(b, n, N_FOURIER_GROUPS, FOURIER_GROUP_DIM).astype(jnp.float32)
    fr = jnp.fft.fft2(fg, axes=(1, 3), norm='ortho').real.astype(f.dtype)
    return jnp.einsum('bngc,gcd->bngd', fr, w_fourier).reshape(b, n, FOURIER_WIDTH)


def hier_moe(h, w_rg, b_rg, w_re, b_re, w_gate, w_up, w_down):
    t, d = h.shape
    hf = h.astype(jnp.float32)
    g_logits = hf @ w_rg.astype(jnp.float32) + b_rg.astype(jnp.float32)
    grp = jnp.argmax(g_logits, axis=-1)
    p_grp = jnp.take_along_axis(jax.nn.softmax(g_logits, axis=-1), grp[:, None], axis=-1)
    e_logits = (hf @ w_re.astype(jnp.float32) + b_re.astype(jnp.float32)).reshape(t, N_GROUPS, EXPERTS_PER_GROUP)
    e_in = jnp.take_along_axis(e_logits, grp[:, None, None], axis=1)[:, 0]
    top_val, top_idx = lax.top_k(e_in, TOP_K)
    gate = p_grp * jax.nn.softmax(top_val, axis=-1)
    expert = grp[:, None] * EXPERTS_PER_GROUP + top_idx

    n_assign = t * TOP_K
    e_flat = expert.reshape(n_assign).astype(jnp.int32)
    tok_flat = jnp.repeat(jnp.arange(t, dtype=jnp.int32), TOP_K)
    w_flat = gate.reshape(n_assign)
    order = jnp.argsort(e_flat)
    e_sorted = e_flat[order]
    counts = jnp.bincount(e_flat, length=N_EXPERTS).astype(jnp.int32)
    starts = jnp.cumsum(counts) - counts
    padded = (counts + MOE_BLOCK - 1) // MOE_BLOCK * MOE_BLOCK
    pad_ends = jnp.cumsum(padded)
    pad_starts = pad_ends - padded
    dest = pad_starts[e_sorted] + jnp.arange(n_assign, dtype=jnp.int32) - starts[e_sorted]
    n_blocks = -(-n_assign // MOE_BLOCK) + N_EXPERTS
    buf_tok = jnp.full((n_blocks * MOE_BLOCK,), t, jnp.int32).at[dest].set(tok_flat[order])
    buf_w = jnp.zeros((n_blocks * MOE_BLOCK,), h.dtype).at[dest].set(w_flat[order].astype(h.dtype))
    block_expert = jnp.minimum(
        jnp.searchsorted(pad_ends, jnp.arange(n_blocks, dtype=jnp.int32) * MOE_BLOCK, side='right'),
        N_EXPERTS - 1)
    h_pad = jnp.concatenate([h, jnp.zeros((1, d), h.dtype)], axis=0)

    def expert_block(args):
        tok, wt, e = args
        xb = h_pad[tok]
        y = (jax.nn.silu(xb @ w_gate[e]) * (xb @ w_up[e])) @ w_down[e]
        return y * wt[:, None]

    ys = lax.map(expert_block, (buf_tok.reshape(n_blocks, MOE_BLOCK),
                                buf_w.reshape(n_blocks, MOE_BLOCK), block_expert))
    out = jnp.zeros((t + 1, d), ys.dtype).at[buf_tok].add(ys.reshape(-1, d))
    return out[:t]


def setup_inputs(seed: int = 0) -> dict:
    key = jax.random.key(seed)
    ks = jax.random.split(key, 24)
    f32 = jnp.float32
    nrm = lambda k, shape, std: jax.random.normal(k, shape, f32) * std
    L = DEPTH
    return {
        'x': nrm(ks[0], (BATCH, SEQ, D_MODEL), 1.0),
        'c': nrm(ks[1], (BATCH, D_MODEL), 1.0),
        'ctx': nrm(ks[2], (BATCH, CTX_LEN, D_MODEL), 1.0),
        'c_ctx': nrm(ks[3], (D_MODEL,), 1.0),
        'w_ada': nrm(ks[4], (L, D_MODEL, N_MOD * D_MODEL), 0.5 * D_MODEL ** -0.5),
        'b_ada': nrm(ks[5], (L, N_MOD * D_MODEL), 0.02),
        'g_mix_norm': 1.0 + nrm(ks[6], (L, D_MODEL), 0.02),
        'g_ffn_norm': 1.0 + nrm(ks[7], (L, D_MODEL), 0.02),
        'w_in': nrm(ks[8], (L, D_MODEL, IN_PROJ_WIDTH), D_MODEL ** -0.5),
        'lambda_q1': nrm(ks[9], (L, DA_HEAD_DIM), 0.1),
        'lambda_k1': nrm(ks[10], (L, DA_HEAD_DIM), 0.1),
        'lambda_q2': nrm(ks[11], (L, DA_HEAD_DIM), 0.1),
        'lambda_k2': nrm(ks[12], (L, DA_HEAD_DIM), 0.1),
        'g_subln': 1.0 + nrm(ks[13], (L, DA_V_DIM), 0.02),
        'w_fourier': nrm(ks[14], (L, N_FOURIER_GROUPS, FOURIER_GROUP_DIM, FOURIER_GROUP_DIM), FOURIER_GROUP_DIM ** -0.5),
        'w_out': nrm(ks[15], (L, MIX_WIDTH, D_MODEL), MIX_WIDTH ** -0.5),
        'w_router_group': nrm(ks[16], (L, D_MODEL, N_GROUPS), D_MODEL ** -0.5),
        'b_router_group': nrm(ks[17], (L, N_GROUPS), 0.01),
        'w_router_expert': nrm(ks[18], (L, D_MODEL, N_EXPERTS), D_MODEL ** -0.5),
        'b_router_expert': nrm(ks[19], (L, N_EXPERTS), 0.01),
        'w_gate': nrm(ks[20], (L, N_EXPERTS, D_MODEL, D_EXPERT), D_MODEL ** -0.5),
        'w_up': nrm(ks[21], (L, N_EXPERTS, D_MODEL, D_EXPERT), D_MODEL ** -0.5),
        'w_down': nrm(ks[22], (L, N_EXPERTS, D_EXPERT, D_MODEL), D_EXPERT ** -0.5),
        'g_final': 1.0 + nrm(ks[23], (D_MODEL,), 0.02),
    }


def reference(x, c, ctx, c_ctx, w_ada, b_ada, g_mix_norm, g_ffn_norm, w_in,
              lambda_q1, lambda_k1, lambda_q2, lambda_k2, g_subln, w_fourier, w_out,
              w_router_group, b_router_group, w_router_expert, b_router_expert,
              w_gate, w_up, w_down, g_final):
    b, s, d = x.shape
    n_ctx = ctx.shape[1]
    ROWS = s // GRID_W
    cos, sin = axial_rope_tables(ROWS, x.dtype)
    o_k = QK_WIDTH
    o_v = 2 * QK_WIDTH
    o_f = 2 * QK_WIDTH + ATTN_WIDTH
    xl, xc = x, ctx
    for l in range(DEPTH):
        last = l == DEPTH - 1
        lambda_init = 0.8 - 0.6 * math.exp(-0.3 * l)
        lam = diff_lambda(lambda_q1[l], lambda_k1[l], lambda_q2[l], lambda_k2[l], lambda_init)
        mod_l = (jax.nn.silu(c) @ w_ada[l] + b_ada[l]).reshape(b, N_MOD, 1, d)
        mod_c = (jax.nn.silu(c_ctx) @ w_ada[l] + b_ada[l]).reshape(N_MOD, d)
        sh1, sc1, gt1, sh2, sc2, gt2 = (mod_l[:, i] for i in range(N_MOD))
        csh1, csc1, cgt1, csh2, csc2, cgt2 = (mod_c[i] for i in range(N_MOD))
        wl = w_in[l]
        moe_args = (w_router_group[l], b_router_group[l], w_router_expert[l], b_router_expert[l],
                    w_gate[l], w_up[l], w_down[l])

        hl = modulate(rms_norm(xl, g_mix_norm[l]), sh1, sc1)
        pl = hl @ wl
        ql = apply_axial_rope(pl[..., :o_k].reshape(b, s, N_DA_HEADS, 2, DA_HEAD_DIM), cos, sin)
        kl = apply_axial_rope(pl[..., o_k:o_v].reshape(b, s, N_DA_HEADS, 2, DA_HEAD_DIM), cos, sin)
        vl = pl[..., o_v:o_f].reshape(b, s, N_DA_HEADS, DA_V_DIM)
        fl = pl[..., o_f:]

        hc = modulate(rms_norm(xc, g_mix_norm[l]), csh1, csc1)
        pc_kv = hc @ wl[:, o_k:o_f]
        kc = pc_kv[..., :QK_WIDTH].reshape(b, n_ctx, N_DA_HEADS, 2, DA_HEAD_DIM)
        vc = pc_kv[..., QK_WIDTH:].reshape(b, n_ctx, N_DA_HEADS, DA_V_DIM)

        k_all = jnp.concatenate([kc, kl], axis=1)
        v_all = jnp.concatenate([vc, vl], axis=1)
        mix_l = jnp.concatenate([diff_attention(ql, k_all, v_all, lam, g_subln[l], lambda_init),
                                 fourier_mix(fl, w_fourier[l])], axis=-1)
        xl = xl + gt1 * (mix_l @ w_out[l])
        h2l = modulate(rms_norm(xl, g_ffn_norm[l]), sh2, sc2)

        if last:
            xl = xl + gt2 * hier_moe(h2l.reshape(b * s, d), *moe_args).reshape(b, s, d)
        else:
            qc = (hc @ wl[:, :o_k]).reshape(b, n_ctx, N_DA_HEADS, 2, DA_HEAD_DIM)
            fc = hc @ wl[:, o_f:]
            mix_c = jnp.concatenate([diff_attention(qc, kc, vc, lam, g_subln[l], lambda_init),
                                     fourier_mix(fc, w_fourier[l])], axis=-1)
            xc = xc + cgt1 * (mix_c @ w_out[l])
            h2c = modulate(rms_norm(xc, g_ffn_norm[l]), csh2, csc2)
            y = hier_moe(jnp.concatenate([h2l.reshape(b * s, d), h2c.reshape(b * n_ctx, d)], axis=0), *moe_args)
            xl = xl + gt2 * y[:b * s].reshape(b, s, d)
            xc = xc + cgt2 * y[b * s:].reshape(b, n_ctx, d)
    return rms_norm(xl, g_final)
```

```python
import contextlib
import numpy as np
import concourse.bass as bass
import concourse.mybir as mybir
from concourse.bass_utils import run_bass_kernel_spmd

F32 = mybir.dt.float32
BF16 = mybir.dt.bfloat16
I32 = mybir.dt.int32
AF = mybir.ActivationFunctionType
ALU = mybir.AluOpType
AX = mybir.AxisListType

D = 1024
NT = 64
NOWN = 16
NKT = 66
CAP = 384
NSLOT = 32 * CAP
EPS = 1e-6
LAMBDA_INIT = 0.2


class FW:
    ENG = ("pe", "act", "dve", "pool", "sp")

    def __init__(self, nc):
        self.nc = nc
        self.ops = {e: [] for e in self.ENG}
        self.seq = {e: 0 for e in self.ENG}
        self.lastc = {}
        self.waited = {e: {} for e in self.ENG}
        self.dma_cnt = {}
        self.dma_rr = {}
        self.lastw = {}
        self.readers = {}
        self.needed = set()

    def _deps(self, eng, reads, writes):
        ev = []
        for r in reads:
            if r in self.lastw:
                ev.append(self.lastw[r])
        for w in writes:
            if w in self.lastw:
                ev.append(self.lastw[w])
            ev.extend(self.readers.get(w, ()))
        best = {}
        for (k, v) in ev:
            if k == eng and eng == "pe":
                continue
            if v > best.get(k, 0):
                best[k] = v
        return self._waits(eng, best)

    def _waits(self, eng, best):
        waits = []
        wd = self.waited[eng]
        for k, v in best.items():
            if wd.get(k, 0) >= v:
                continue
            wd[k] = v
            waits.append((k, v))
            if k in self.ENG:
                self.needed.add((k, v))
        return waits

    def _mark(self, token, reads, writes):
        for r in reads:
            self.readers.setdefault(r, []).append(token)
        for w in writes:
            self.lastw[w] = token
            self.readers[w] = []

    def op(self, eng, fn, reads=(), writes=()):
        waits = self._deps(eng, reads, writes)
        self.seq[eng] += 1
        tok = (eng, self.seq[eng])
        self.lastc[eng] = tok
        self.ops[eng].append([fn, waits, "c", tok])
        self._mark(tok, reads, writes)
        return tok

    NSEM = 20

    def dma(self, q, stream, fn, reads=(), writes=()):
        r = self.dma_rr.get(q, 0)
        self.dma_rr[q] = (r + 1) % self.NSEM
        key = "dq_%s_%d" % (q, r)
        prev = self.dma_cnt.get(key, 0)
        waits = self._deps(q, reads, writes)
        if prev:
            waits = waits + self._waits(q, {key: prev})
        self.dma_cnt[key] = prev + 16
        tok = (key, prev + 16)
        self.ops[q].append([fn, waits, "d", tok])
        self._mark(tok, reads, writes)
        return tok

    def barrier(self, keep=()):
        best = {k: v for (k, v) in self.lastc.values()}
        for k, v in self.dma_cnt.items():
            best[k] = v
        kept = {}
        for key in keep:
            if key in self.lastw:
                tk = self.lastw[key]
                kept[key] = tk
                if tk[0] in self.dma_cnt and best.get(tk[0], 0) >= tk[1]:
                    best[tk[0]] = tk[1] - 16
        for e in self.ENG:
            b = {k: v for k, v in best.items() if k != e and v > 0}
            self.ops[e].append([None, self._waits(e, b), "w", None])
        self.lastw = dict(kept)
        self.readers = {k: [] for k in kept}

    def final_wait(self, eng, keys):
        waits = self._deps(eng, list(keys), [])
        self.ops[eng].append([None, waits, "w", None])

    def emit(self, stack):
        nc = self.nc
        sems = {}
        for e in self.ENG:
            sems[e] = stack.enter_context(nc.semaphore("s_" + e))
        for k in self.dma_cnt:
            sems[k] = stack.enter_context(nc.semaphore(k))
        sigval = {}
        for e in self.ENG:
            c = 0
            for o in self.ops[e]:
                if o[2] == "c" and o[3] in self.needed:
                    c += 1
                    sigval[o[3]] = c
        block = stack.enter_context(nc.Block())

        def run(e, h):
            for fn, waits, kind, tok in self.ops[e]:
                for (k, v) in waits:
                    h.wait_ge(sems[k], sigval[(k, v)] if k in self.ENG else v)
                if kind == "w":
                    continue
                ins = fn(h)
                if kind == "d":
                    ins.then_inc(sems[tok[0]], 16)
                elif tok in sigval:
                    ins.then_inc(sems[e], 1)

        @block.tensor
        def _(h):
            run("pe", h)

        @block.scalar
        def _(h):
            run("act", h)

        @block.vector
        def _(h):
            run("dve", h)

        @block.gpsimd
        def _(h):
            run("pool", h)

        @block.sync
        def _(h):
            run("sp", h)


def build_nc(debug=False):
    nc = bass.Bass("TRN2", target_bir_lowering=False)

    def din(name, shape, dt=F32):
        return nc.dram_tensor(name, list(shape), dt, kind="ExternalInput").ap()

    x_d = din("x", [NT, 128, D])
    ctx_d = din("ctx", [2, 128, D])
    ccT_d = din("ccT", [128, 8, 2])
    wada_d = din("w_ada", [D, 6 * D])
    badaT_d = din("badaT", [128, 6, 8])
    bada_d = din("b_ada", [1, 6 * D])
    gmixT_d = din("gmixT", [128, 8])
    gffn_d = din("g_ffn", [1, D])
    gfin_d = din("g_final", [1, D])
    win_d = din("w_in", [D, 2048])
    lam_d = din("lam4", [1, 256])
    gsub_d = din("g_subln", [1, 128])
    wf_d = din("w_fourier", [4, 128, 128])
    wout_d = din("w_out", [D, D])
    wr_d = din("w_router", [D, 36])
    br_d = din("b_router", [1, 36])
    wg_d = din("w_gate", [32, D, 512])
    wu_d = din("w_up", [32, D, 512])
    wd_d = din("w_down", [32, 512, D])
    ident_d = din("ident", [128, 128])
    rope_d = din("rope", [NT, 128, 128])
    mb_d = din("mb", [128, NT, 64])
    eb_d = din("eb", [3, 128, 128])
    cs_d = din("cs", [2, 128, 128])
    lst_d = din("lstrict", [128, 128])
    iota_d = din("iota", [128, 32])
    y_d = nc.dram_tensor("y", [NOWN, 128, D], F32, kind="ExternalOutput").ap()

    if debug:
        dbg_attn = nc.dram_tensor("dbg_attn", [128, NOWN, 512], F32, kind="ExternalOutput").ap()
        dbg_fo = nc.dram_tensor("dbg_fo", [128, 4, 2048], F32, kind="ExternalOutput").ap()
        dbg_xl = nc.dram_tensor("dbg_xl", [NOWN, 128, D], F32, kind="ExternalOutput").ap()
        dbg_gates = nc.dram_tensor("dbg_gates", [128, NOWN, 2], F32, kind="ExternalOutput").ap()
        dbg_dest = nc.dram_tensor("dbg_dest", [128, NOWN, 2, 2], I32, kind="ExternalOutput").ap()
        dbg_q = nc.dram_tensor("dbg_q", [128, 4, 2048], F32, kind="ExternalOutput").ap()
    vscr = nc.dram_tensor("vscr", [128, NKT, 4 * 129], BF16, kind="Internal").ap()
    zscr = nc.dram_tensor("zscr", [64, NT, 512], BF16, kind="Internal").ap()
    xslots = nc.dram_tensor("xslots", [NSLOT, D], BF16, kind="Internal").ap()
    ys = nc.dram_tensor("ys", [NSLOT + 1, D], F32, kind="Internal").ap()
    xlscr = nc.dram_tensor("xlscr", [NOWN, 128, D], F32, kind="Internal").ap()

    fw = FW(nc)
    with contextlib.ExitStack() as st:
        USZ = 168 * 1024
        U = st.enter_context(nc.sbuf_tensor("U", [128, USZ // 2], BF16))

        def carve(off, dims, dt, parts=128):
            esz = 4 if dt in (F32, I32) else 2
            n = int(np.prod(dims)) * esz
            assert off % 4 == 0 and off + n <= USZ, (off, n)
            ap = U[0:parts, off // 2:(off + n) // 2]
            if dt != BF16:
                ap = ap.bitcast(dt)
            if len(dims) > 1:
                names = " ".join("d%d" % i for i in range(len(dims)))
                kw = {"d%d" % i: dims[i] for i in range(len(dims))}
                ap = ap.rearrange("p (%s) -> p %s" % (names, names), **kw)
            return ap

        def T(name, shape, dt):
            return st.enter_context(nc.sbuf_tensor("sb_" + name, list(shape), dt))

        PS = [st.enter_context(nc.psum_tensor("ps%d" % i, [128, 2, 512], F32)) for i in range(4)]

        def bank(i):
            return PS[i // 2][:, i % 2, :]

        def bank_bf(i):
            return PS[i // 2][:, i % 2, :].bitcast(BF16)

        identF = T("identF", [128, 128], F32)
        identB = T("identB", [128, 128], BF16)
        ones_f = T("ones_f", [128, 128], F32)
        ones_b = T("ones_b", [128, 128], BF16)
        lstB = T("lstB", [128, 128], BF16)
        iotaF = T("iotaF", [128, 32], F32)
        ccT = T("ccT", [128, 8, 2], F32)
        scT = T("scT", [128, 8, 2], F32)
        badaT = T("badaT", [128, 6, 8], F32)
        gmixT = T("gmixT", [128, 8], F32)
        modT = T("modT", [128, 2, 8, 2], F32)
        amod = T("amod", [128, 2, 8], F32)
        smod = T("smod", [128, 2, 8], F32)
        stat = T("stat", [128, NKT, 4], F32)
        lamt = T("lamt", [128, 256], F32)
        lamw = T("lamw", [128, 8], F32)
        gsub = T("gsub", [128, 128], F32)
        attn_tok = T("attn_tok", [128, NOWN, 512], BF16)
        small = T("small", [128, 64], F32)
        AB = T("AB", [128, 2, 4, 128], BF16)
        EB = T("EB", [128, 3, 128], BF16)
        gates = T("gates", [128, NOWN, 2], F32)
        desti = T("desti", [128, NOWN, 2, 2], I32)
        Bt = T("Bt", [128, NOWN, 32], BF16)
        rt = T("rt", [128, 13, 36], F32)
        wr = T("wr", [128, 8, 36], F32)
        brow = T("brow", [1, 36], F32)

        dq = {"n": 0}

        def ld(out, in_, key, q="sp", stream="ld"):
            return fw.dma(q, stream, lambda h: h.dma_start(out=out, in_=in_), writes=[key])

        ld(identF[:], ident_d, "identF")
        ld(identB[:], ident_d, "identB", q="pool", stream="cast")
        ld(lstB[:], lst_d, "lstB", q="pool", stream="cast")
        ld(EB[:], eb_d.rearrange("e p n -> p e n"), "EB", q="pool", stream="cast")
        ld(iotaF[:], iota_d, "iotaF")
        ld(ccT[:], ccT_d, "ccT")
        ld(badaT[:], badaT_d, "badaT")
        ld(gmixT[:], gmixT_d, "gmixT")
        ld(lamt[:], lam_d.partition_broadcast(128)[:, 0, :], "lamt")
        ld(gsub[:], gsub_d.partition_broadcast(128)[:, 0, :], "gsub")
        ld(wr[:], wr_d.rearrange("(c p) n -> p c n", p=128), "wr")
        ld(brow[:], br_d, "brow")
        fw.op("dve", lambda h: h.memset(ones_f[:], 1.0), writes=["ones_f"])
        fw.op("dve", lambda h: h.memset(stat[:], 0.0), writes=["stat%d" % t for t in range(NKT)])
        fw.op("dve", lambda h: h.memset(small[:], 0.0), writes=["small"])
        fw.op("dve", lambda h: h.memset(lamw[:], 0.0), writes=["lamw"])
        fw.op("dve", lambda h: h.memset(ones_b[:], 1.0), writes=["ones_b"])
        fw.op("act", lambda h: h.activation(out=scT[:], in_=ccT[:], func=AF.Silu), reads=["ccT"], writes=["scT"])
        lt = lamt[:].rearrange("p (k d) -> p k d", k=4)
        fw.op("dve", lambda h: h.tensor_tensor(out=small[:, 0:64], in0=lt[:, 0, :], in1=lt[:, 1, :], op=ALU.mult), reads=["lamt"], writes=["small"])
        fw.op("dve", lambda h: h.reduce_sum(out=lamw[:, 0:1], in_=small[:, 0:64], axis=AX.X), reads=["small"], writes=["lamw"])
        fw.op("dve", lambda h: h.tensor_tensor(out=small[:, 0:64], in0=lt[:, 2, :], in1=lt[:, 3, :], op=ALU.mult), reads=["lamt", "lamw"], writes=["small"])
        fw.op("dve", lambda h: h.reduce_sum(out=lamw[:, 1:2], in_=small[:, 0:64], axis=AX.X), reads=["small"], writes=["lamw"])
        fw.op("act", lambda h: h.activation(out=lamw[:, 2:4], in_=lamw[:, 0:2], func=AF.Exp), reads=["lamw"], writes=["lamw"])
        fw.op("dve", lambda h: h.tensor_tensor(out=lamw[:, 4:5], in0=lamw[:, 3:4], in1=lamw[:, 2:3], op=ALU.subtract), reads=["lamw"], writes=["lamw"])
        fw.op("dve", lambda h: h.tensor_scalar(out=lamw[:, 4:5], in0=lamw[:, 4:5], scalar1=-LAMBDA_INIT, scalar2=0.0, op0=ALU.add, op1=ALU.add), reads=["lamw"], writes=["lamw"])
        fw.op("dve", lambda h: h.tensor_scalar(out=gsub[:], in0=gsub[:], scalar1=1.0 - LAMBDA_INIT, scalar2=0.0, op0=ALU.mult, op1=ALU.add), reads=["gsub"], writes=["gsub"])

        O_POST_C = 101376
        O_KT = 0
        O_QT = O_KT + 4 * 8448 * 2
        O_WIN = O_QT + 4 * 2048 * 2
        O_W = O_WIN + 8 * 2048 * 2
        KT = carve(O_KT, [4, 8448], BF16)
        QT = carve(O_QT, [4, 2048], BF16)
        Win = carve(O_WIN, [8, 2048], BF16)
        o = O_W
        Mb = carve(o, [NT, 64], BF16); o += NT * 64 * 2
        wa = carve(o, [8, 1024], F32)
        xt = [carve(o + i * 4096, [1024], F32) for i in range(2)]; o += 8192
        xn = [carve(o + i * 2048, [1024], BF16) for i in range(2)]; o += 4096
        hlT = [carve(o + i * 2048, [8, 128], BF16) for i in range(2)]; o += 4096
        rp = [carve(o + i * 512, [128], F32) for i in range(3)]; o += 1536
        tcs = carve(o, [2, 512], F32); o += 4096
        ktok = [carve(o + i * 1024, [512], BF16) for i in range(2)]; o += 2048
        qtok = [carve(o + i * 1024, [512], BF16) for i in range(2)]; o += 2048
        vst = [carve(o + i * 1032, [4, 129], BF16) for i in range(2)]; o += 2064
        fb = [carve(o + i * 1024, [512], BF16) for i in range(2)]; o += 2048
        zst = [carve(o + i * 4096, [4, 512], BF16, parts=64) for i in range(2)]; o += 8192
        junk = carve(o, [1024], BF16); o += 2048
        assert o <= USZ, o

        for c in range(8):
            fw.dma("pool", "cast", lambda h, c=c: h.dma_start(out=Win[:, c, :], in_=win_d[c * 128:(c + 1) * 128, :]), writes=["Win"])
        ld(Mb, mb_d, "Mb", q="pool", stream="cast")

        for mod in range(2):
            for c in range(8):
                fw.dma("sp", "ld", lambda h, c=c, mod=mod: h.dma_start(out=wa[:, c, :], in_=wada_d[c * 128:(c + 1) * 128, mod * D:(mod + 1) * D]), writes=["wa"])
            pm = bank(7)
            for mc in range(8):
                for kc in range(8):
                    fw.op("pe", lambda h, mc=mc, kc=kc: h.matmul(pm[:, mc * 2:mc * 2 + 2], lhsT=wa[:, kc, mc * 128:(mc + 1) * 128], rhs=scT[:, kc, :], start=(kc == 0), stop=(kc == 7)),
                          reads=["wa", "scT"], writes=["b7"])
            fw.op("dve", lambda h, mod=mod: h.tensor_copy(out=modT[:, mod, :, :], in_=pm[:, 0:16].rearrange("p (a b) -> p a b", b=2)), reads=["b7"], writes=["modT"])
        for mod in range(2):
            for j in range(2):
                fw.op("dve", lambda h, mod=mod, j=j: h.tensor_tensor(out=modT[:, mod, :, j], in0=modT[:, mod, :, j], in1=badaT[:, mod, :], op=ALU.add), reads=["modT", "badaT"], writes=["modT"])
        for j in range(2):
            fw.op("dve", lambda h, j=j: h.scalar_tensor_tensor(out=amod[:, j, :], in0=modT[:, 1, :, j], scalar=1.0, in1=gmixT[:], op0=ALU.add, op1=ALU.mult), reads=["modT", "gmixT"], writes=["amod"])
            fw.op("dve", lambda h, j=j: h.tensor_copy(out=smod[:, j, :], in_=modT[:, 0, :, j]), reads=["modT"], writes=["smod"])

        fw.barrier()
        for i in range(2):
            fw.op("dve", lambda h, i=i: h.memset(vst[i][:, :, 128:129], 1.0), writes=["vst%d" % i])
        B_T, B_K, B_Q, B_V, B_F, B_Z, B_X = 0, 1, 2, 3, 4, 5, 6

        def tinfo(t):
            lat = t >= 2
            u = t - 2
            return lat, u, (lat and u < NOWN), t % 2

        def st1(t):
            lat, u, own, s = tinfo(t)
            src = x_d[u] if lat else ctx_d[t]
            fw.dma("sp", "ld", lambda h: h.dma_start(out=xt[s], in_=src), writes=["xt%d" % s])
            if lat:
                fw.dma("sp", "ld", lambda h: h.dma_start(out=rp[t % 3], in_=rope_d[u]), writes=["rp%d" % (t % 3)])
            fw.op("act", lambda h: h.activation(out=junk, in_=xt[s], func=AF.Square, accum_out=stat[:, t, 0:1]), reads=["xt%d" % s], writes=["junk", "stat%d" % t])
            fw.op("dve", lambda h: h.tensor_scalar(out=stat[:, t, 1:2], in0=stat[:, t, 0:1], scalar1=1.0 / D, scalar2=EPS, op0=ALU.mult, op1=ALU.add), reads=["stat%d" % t], writes=["stat%d" % t])
            fw.op("act", lambda h: h.activation(out=stat[:, t, 2:3], in_=stat[:, t, 1:2], func=AF.Sqrt), reads=["stat%d" % t], writes=["stat%d" % t])
            fw.op("dve", lambda h: h.reciprocal(out=stat[:, t, 3:4], in_=stat[:, t, 2:3]), reads=["stat%d" % t], writes=["stat%d" % t])
            fw.op("act", lambda h: h.activation(out=xn[s], in_=xt[s], func=AF.Copy, scale=stat[:, t, 3:4]), reads=["xt%d" % s, "stat%d" % t], writes=["xn%d" % s])

        def st1b(t):
            lat, u, own, s = tinfo(t)
            pT = bank_bf(B_T).rearrange("p (c n) -> p c n", c=8)
            for c in range(8):
                fw.op("pe", lambda h, c=c: h.transpose(out=pT[:, c, :], in_=xn[s][:, c * 128:(c + 1) * 128], identity=identB[:]), reads=["xn%d" % s, "identB"], writes=["bT"])
            jj = 0 if lat else 1
            for c in range(8):
                fw.op("dve", lambda h, c=c: h.tensor_scalar(out=hlT[s][:, c, :], in0=pT[:, c, :], scalar1=amod[:, jj, c:c + 1], scalar2=smod[:, jj, c:c + 1], op0=ALU.mult, op1=ALU.add),
                      reads=["bT", "amod", "smod"], writes=["hlT%d" % s])

        def rope(bk, key, dst, dkey, r):
            s = r
            xv = bank(bk)
            x3 = xv.rearrange("p (g d) -> p g d", g=8)
            cosb = rp[s][:, 0:64].unsqueeze(1).to_broadcast([128, 8, 64])
            sinv = rp[s][:, 64:128].rearrange("p (a t j) -> p a t j", a=2, t=2)
            x5 = xv.rearrange("p (g a t j) -> p g a t j", g=8, a=2, t=2)
            tc3 = tcs[:, 0, :].rearrange("p (g d) -> p g d", g=8)
            ts5 = tcs[:, 1, :].rearrange("p (g a t j) -> p g a t j", g=8, a=2, t=2)
            fw.op("dve", lambda h: h.tensor_tensor(out=tc3, in0=x3, in1=cosb, op=ALU.mult), reads=[key, "rp%d" % s], writes=["tcs0"])
            for tt in range(2):
                sb_ = sinv[:, :, tt, :].unsqueeze(1).to_broadcast([128, 8, 2, 16])
                fw.op("dve", lambda h, tt=tt, sb_=sb_: h.tensor_tensor(out=ts5[:, :, :, tt, :], in0=x5[:, :, :, 1 - tt, :], in1=sb_, op=ALU.mult), reads=[key, "rp%d" % s], writes=["tcs1"])
            fw.op("dve", lambda h: h.tensor_tensor(out=dst, in0=tcs[:, 0, :], in1=tcs[:, 1, :], op=ALU.add), reads=["tcs0", "tcs1"], writes=[dkey])

        def st2(t):
            lat, u, own, s = tinfo(t)
            blocks = [(B_K, 512, "bK"), (B_V, 1024, "bV")]
            if lat:
                blocks.append((B_F, 1536, "bF"))
            if own:
                blocks.append((B_Q, 0, "bQ"))
            for (bk, col, key) in blocks:
                for c in range(8):
                    fw.op("pe", lambda h, c=c, bk=bk, col=col: h.matmul(bank(bk), lhsT=hlT[s][:, c, :], rhs=Win[:, c, col:col + 512], start=(c == 0), stop=(c == 7)),
                          reads=["hlT%d" % s, "Win"], writes=[key])
            if lat:
                rope(B_K, "bK", ktok[s], "ktok%d" % s, t % 3)
            else:
                fw.op("dve", lambda h: h.tensor_copy(out=ktok[s], in_=bank(B_K)), reads=["bK"], writes=["ktok%d" % s])
            if own:
                rope(B_Q, "bQ", qtok[s], "qtok%d" % s, t % 3)
            fw.op("act", lambda h: h.copy(out=vst[s][:, :, 0:128], in_=bank(B_V).rearrange("p (g d) -> p g d", g=4)), reads=["bV"], writes=["vst%d" % s])
            fw.dma("sp", "st", lambda h: h.dma_start(out=vscr[:, t, :], in_=vst[s].rearrange("p g d -> p (g d)")), reads=["vst%d" % s], writes=["vscr"])
            if lat:
                fw.op("act", lambda h: h.copy(out=fb[s], in_=bank(B_F)), reads=["bF"], writes=["fb%d" % s])

        def st3(t):
            lat, u, own, s = tinfo(t)
            pX = bank_bf(B_X).rearrange("p (c n) -> p c n", c=8)
            for hh in range(4):
                fw.op("pe", lambda h, hh=hh: h.transpose(out=pX[:, hh, :], in_=ktok[s][:, hh * 128:(hh + 1) * 128], identity=identB[:]), reads=["ktok%d" % s, "identB"], writes=["bX"])
            if own:
                for hh in range(4):
                    fw.op("pe", lambda h, hh=hh: h.transpose(out=pX[:, 4 + hh, :], in_=qtok[s][:, hh * 128:(hh + 1) * 128], identity=identB[:]), reads=["qtok%d" % s, "identB"], writes=["bX"])
            fw.op("act", lambda h: h.copy(out=KT[:, :, t * 128:(t + 1) * 128], in_=pX[:, 0:4, :]), reads=["bX"], writes=["KT"])
            if own:
                fw.op("act", lambda h: h.copy(out=QT[:, :, u * 128:(u + 1) * 128], in_=pX[:, 4:8, :]), reads=["bX"], writes=["QT"])
            if lat:
                fw.op("pe", lambda h: h.matmul(bank(B_Z)[0:64, :], lhsT=Mb[:, u, :], rhs=fb[s], start=True, stop=True), reads=["fb%d" % s, "Mb"], writes=["bZ"])
                zs = (u // 4) % 2
                fw.op("dve", lambda h: h.tensor_copy(out=zst[zs][:, u % 4, :], in_=bank(B_Z)[0:64, :]), reads=["bZ"], writes=["zst%d" % zs])
                if u % 4 == 3:
                    fw.dma("sp", "st", lambda h: h.dma_start(out=zscr[:, u - 3:u + 1, :], in_=zst[zs]), reads=["zst%d" % zs], writes=["zscr"])

        st1(0)
        for step in range(NKT + 2):
            if step + 1 < NKT:
                st1(step + 1)
            if step < NKT:
                st1b(step)
            if 1 <= step <= NKT:
                st2(step - 1)
            if step >= 2:
                st3(step - 2)

        fw.barrier()
        if debug:
            fw.dma("pool", "dbg", lambda h: h.dma_start(out=dbg_q, in_=QT), reads=["QT"], writes=["dbg_q"])
        O_VA = O_WIN
        o = O_VA
        VA = carve(o, [NKT, 4, 129], BF16); o += NKT * 4 * 129 * 2
        o = (o + 3) // 4 * 4
        ET = [carve(o + i * 2048, [2, 512], BF16) for i in range(2)]; o += 4096
        qp = [[carve(o + (i * 2 + m) * 1024, [512], BF16) for m in range(2)] for i in range(2)]; o += 4096
        osb = carve(o, [4, 128], F32); o += 2048
        osq = carve(o, [128], F32); o += 512
        Ocp = carve(o, [4, 512], F32); o += 8192
        assert o <= USZ, o
        for g in range(6):
            fw.dma("sp", "ld", lambda h, g=g: h.dma_start(out=VA[:, g * 11:(g + 1) * 11, :, :].rearrange("p t g d -> p t (g d)"), in_=vscr[:, g * 11:(g + 1) * 11, :]), reads=["vscr"], writes=["VA"])
        for i in range(2):
            for m in range(2):
                fw.op("pool", lambda h, i=i, m=m: h.memset(qp[i][m], 0.0), writes=["qp%d" % i])
        SB = [PS[0], PS[1]]
        OB = [PS[2], PS[3]]

        def Oap(m, j):
            return OB[m][:, j // 2, (j % 2) * 256:(j % 2) * 256 + 129]

        iters = [(qb, hh, kt) for qb in range(4) for hh in range(4) for kt in range(NKT)]

        def emit_qp(qb, hh):
            pass

        def emit_S(n):
            qb, hh, kt = iters[n]
            sb = n % 2
            for m in range(2):
                fw.op("pe", lambda h, m=m: h.matmul(SB[sb][:, m, :], lhsT=KT[m * 64:(m + 1) * 64, hh, kt * 128:(kt + 1) * 128], rhs=QT[m * 64:(m + 1) * 64, hh, qb * 512:(qb + 1) * 512], start=True, stop=True, tile_position=(m * 64, 0)),
                      reads=["KT", "QT"], writes=["S%d" % sb])

        def emit_exp(n):
            sb = n % 2
            fw.op("act", lambda h: h.activation(out=ET[sb], in_=SB[sb][:, :, :], func=AF.Exp, scale=0.125), reads=["S%d" % sb], writes=["ET%d" % sb])

        def emit_AV(n):
            qb, hh, kt = iters[n]
            sb = n % 2
            for m in range(2):
                for j in range(4):
                    fw.op("pe", lambda h, m=m, j=j: h.matmul(Oap(m, j), lhsT=ET[sb][:, m, j * 128:(j + 1) * 128], rhs=VA[:, kt, hh, :], start=(kt == 0 and j % 2 == 0), stop=(kt == NKT - 1), skip_group_check=True),
                          reads=["ET%d" % sb, "VA"], writes=["O"])

        def Ocp_ap(m, j):
            return Ocp[:, m * 2 + j // 2, (j % 2) * 256:(j % 2) * 256 + 129]

        def emit_norm1(qb, hh):
            for bk in range(4):
                fw.op("dve", lambda h, bk=bk: h.tensor_copy(out=Ocp[:, bk, :], in_=OB[bk // 2][:, bk % 2, :]), reads=["O"], writes=["Ocp"])
            for j in range(4):
                fw.op("dve", lambda h, j=j: h.reciprocal(out=small[:, 0:1], in_=Ocp_ap(0, j)[:, 128:129]), reads=["Ocp"], writes=["small"])
                fw.op("dve", lambda h, j=j: h.reciprocal(out=small[:, 1:2], in_=Ocp_ap(1, j)[:, 128:129]), reads=["Ocp"], writes=["small"])
                fw.op("dve", lambda h: h.tensor_tensor(out=small[:, 2:3], in0=small[:, 1:2], in1=lamw[:, 4:5], op=ALU.mult), reads=["small", "lamw"], writes=["small"])
                fw.op("dve", lambda h, j=j: h.tensor_scalar(out=osb[:, j, :], in0=Ocp_ap(0, j)[:, 0:128], scalar1=small[:, 0:1], scalar2=0.0, op0=ALU.mult, op1=ALU.add), reads=["Ocp", "small"], writes=["osb"])
                fw.op("dve", lambda h, j=j: h.scalar_tensor_tensor(out=osb[:, j, :], in0=Ocp_ap(1, j)[:, 0:128], scalar=small[:, 2:3], in1=osb[:, j, :], op0=ALU.mult, op1=ALU.add), reads=["Ocp", "small", "osb"], writes=["osb"])
                fw.op("dve", lambda h, j=j: h.tensor_tensor(out=osq, in0=osb[:, j, :], in1=osb[:, j, :], op=ALU.mult), reads=["osb"], writes=["osq"])
                fw.op("dve", lambda h, j=j: h.reduce_sum(out=small[:, 8 + j:9 + j], in_=osq, axis=AX.X), reads=["osq"], writes=["small"])
            fw.op("dve", lambda h: h.tensor_scalar(out=small[:, 12:16], in0=small[:, 8:12], scalar1=1.0 / 128, scalar2=EPS, op0=ALU.mult, op1=ALU.add), reads=["small"], writes=["small2"])

        def emit_norm2(qb, hh):
            fw.op("act", lambda h: h.activation(out=small[:, 16:20], in_=small[:, 12:16], func=AF.Ln), reads=["small2"], writes=["small3"])
            fw.op("act", lambda h: h.activation(out=small[:, 20:24], in_=small[:, 16:20], func=AF.Exp, scale=-0.5), reads=["small3"], writes=["small3"])
            for j in range(4):
                qt = qb * 4 + j
                fw.op("dve", lambda h, qt=qt, j=j: h.scalar_tensor_tensor(out=attn_tok[:, qt, hh * 128:(hh + 1) * 128], in0=osb[:, j, :], scalar=small[:, 20 + j:21 + j], in1=gsub[:], op0=ALU.mult, op1=ALU.mult),
                      reads=["osb", "small3", "gsub"], writes=["attn_tok"])

        emit_S(0)
        pending = None
        for n in range(len(iters)):
            qb, hh, kt = iters[n]
            emit_exp(n)
            if n + 1 < len(iters):
                emit_S(n + 1)
            emit_AV(n)
            if kt == 12 and pending is not None:
                emit_norm2(*pending)
                pending = None
            if kt == NKT - 1:
                emit_norm1(qb, hh)
                pending = (qb, hh)
        emit_norm2(*pending)

        fw.barrier()
        if debug:
            fw.dma("pool", "dbg", lambda h: h.dma_start(out=dbg_attn, in_=attn_tok[:]), reads=["attn_tok"], writes=["dbg_attn"])
        wa2b = [carve(O_POST_C + m * 16384, [8, 1024], BF16) for m in range(4)]
        WKEEP = ["wa2b%d" % m for m in range(4)]
        for m in range(4):
            fw.dma("pool", "cast", lambda h, m=m: h.dma_start(out=wa2b[m], in_=wada_d[:, (m + 2) * D:(m + 3) * D].rearrange("(c p) n -> p c n", p=128)), writes=["wa2b%d" % m])
        o = 0
        ZT = carve(o, [2, 16, 512], BF16); o += 32768
        PTt = carve(o, [2, 4, 2048], BF16); o += 32768
        mixT = carve(o, [8, 2048], BF16); o += 32768
        csF = carve(o, [2, 128], F32); o += 1024
        wfF = carve(o, [4, 128], F32); o += 2048
        O_POST = o
        assert O_POST == O_POST_C, O_POST
        for hh in range(2):
            for ri in range(2):
                r0 = ri * 32 + hh * 16
                fw.dma("sp", "ld", lambda h, hh=hh, ri=ri, r0=r0: h.dma_start(out=ZT[hh * 64:(hh + 1) * 64, ri, :, :], in_=zscr[r0:r0 + 16, :, :].rearrange("i b c -> b i c")), reads=["zscr"], writes=["ZT"])
        ld(csF, cs_d.rearrange("e p n -> p e n"), "csF")
        ld(wfF, wf_d.rearrange("g p n -> p g n"), "wfF")
        for g in range(4):
            for e in range(2):
                fw.op("pe", lambda h, g=g, e=e: h.matmul(PS[(g * 2 + e) // 4][:, 0, ((g * 2 + e) % 4) * 128:((g * 2 + e) % 4) * 128 + 128], lhsT=csF[:, e, :], rhs=wfF[:, g, :], start=True, stop=True),
                      reads=["csF", "wfF"], writes=["pAB"])
        for g in range(4):
            for e in range(2):
                k = g * 2 + e
                fw.op("dve", lambda h, g=g, e=e, k=k: h.tensor_copy(out=AB[:, e, g, :], in_=PS[k // 4][:, 0, (k % 4) * 128:(k % 4) * 128 + 128]), reads=["pAB"], writes=["AB"])
        fw.barrier(keep=WKEEP)
        pairs = [((0, 0), (1, 1)), ((0, 2), (1, 0))]
        for i in range(16):
            pb = i % 2
            for e in range(2):
                for g in range(4):
                    for n, (ri, eb) in enumerate(pairs[e]):
                        fw.op("pe", lambda h, pb=pb, e=e, g=g, ri=ri, eb=eb, n=n, i=i: h.matmul(PS[pb * 2 + e][:, 0, g * 128:(g + 1) * 128], lhsT=ZT[:, ri, i, g * 128:(g + 1) * 128], rhs=EB[:, eb, :], start=(n == 0), stop=(n == 1)),
                              reads=["ZT", "EB"], writes=["pPT%d%d" % (pb, e)])
                eng = "dve" if e == 0 else "act"
                if e == 0:
                    fw.op("dve", lambda h, pb=pb, e=e, i=i: h.tensor_copy(out=PTt[:, e, :, i * 128:(i + 1) * 128], in_=PS[pb * 2 + e][:, 0, :].rearrange("p (g a) -> p g a", g=4)), reads=["pPT%d%d" % (pb, e)], writes=["PTt"])
                else:
                    fw.op("act", lambda h, pb=pb, e=e, i=i: h.copy(out=PTt[:, e, :, i * 128:(i + 1) * 128], in_=PS[pb * 2 + e][:, 0, :].rearrange("p (g a) -> p g a", g=4)), reads=["pPT%d%d" % (pb, e)], writes=["PTt"])
        fw.barrier(keep=WKEEP)
        for g in range(4):
            for tb in range(4):
                k = (g * 4 + tb) % 4
                for e in range(2):
                    fw.op("pe", lambda h, g=g, tb=tb, e=e, k=k: h.matmul(PS[k][:, 0, :], lhsT=AB[:, e, g, :], rhs=PTt[:, e, g, tb * 512:(tb + 1) * 512], start=(e == 0), stop=(e == 1)),
                          reads=["AB", "PTt"], writes=["pR%d" % k])
                if tb % 2 == 0:
                    fw.op("dve", lambda h, g=g, tb=tb, k=k: h.tensor_copy(out=mixT[:, 4 + g, tb * 512:(tb + 1) * 512], in_=PS[k][:, 0, :]), reads=["pR%d" % k], writes=["mixT"])
                else:
                    fw.op("act", lambda h, g=g, tb=tb, k=k: h.copy(out=mixT[:, 4 + g, tb * 512:(tb + 1) * 512], in_=PS[k][:, 0, :]), reads=["pR%d" % k], writes=["mixT"])
        for i in range(16):
            pbk = 1 + 2 * (i % 2)
            pv = bank_bf(pbk).rearrange("p (c n) -> p c n", c=8)
            for hh in range(4):
                fw.op("pe", lambda h, i=i, hh=hh, pv=pv: h.transpose(out=pv[:, hh, :], in_=attn_tok[:, i, hh * 128:(hh + 1) * 128], identity=identB[:]), reads=["attn_tok", "identB"], writes=["pAT%d" % (i % 2)])
            fw.op("act" if i % 2 else "dve", (lambda h, i=i, pv=pv: h.copy(out=mixT[:, 0:4, i * 128:(i + 1) * 128], in_=pv[:, 0:4, :])) if i % 2 else (lambda h, i=i, pv=pv: h.tensor_copy(out=mixT[:, 0:4, i * 128:(i + 1) * 128], in_=pv[:, 0:4, :])),
                  reads=["pAT%d" % (i % 2)], writes=["mixT"])
        fw.barrier(keep=WKEEP)

        if debug:
            fw.dma("pool", "dbg", lambda h: h.dma_start(out=dbg_fo, in_=mixT[:, 4:8, :]), reads=["mixT"], writes=["dbg_fo"])
            fw.barrier(keep=WKEEP)
        o = 32768
        Smat = carve(o, [8, 128], BF16); o += 4096
        btmp = carve(o, [1024], F32); o += 4096
        assert o <= 40960
        bc = {}
        for nm in ("gt1", "sh2", "a2", "gt2", "gfin"):
            bc[nm] = carve(o, [1024], F32); o += 4096
        Lall = carve(o, [NOWN, 36], F32); o += 4096
        assert o <= 65536
        Wout = carve(0, [8, 1024], BF16)
        for c in range(8):
            fw.dma("pool", "cast", lambda h, c=c: h.dma_start(out=Wout[:, c, :], in_=wout_d[c * 128:(c + 1) * 128, :]), writes=["Wout"])
        xlb = [carve(O_POST + 32768 + k * 4096, [1024], F32) for k in range(3)]
        for c in range(8):
            fw.op("dve", lambda h, c=c: h.tensor_scalar(out=Smat[:, c, :], in0=ones_f[:], scalar1=scT[:, c, 0:1], scalar2=0.0, op0=ALU.mult, op1=ALU.add), reads=["ones_f", "scT"], writes=["Smat"])
        ld(bc["gfin"], gfin_d.partition_broadcast(128)[:, 0, :], "bc_gfin")
        ld(bc["a2"], gffn_d.partition_broadcast(128)[:, 0, :], "bc_a2")
        for (mod, nm) in ((2, "gt1"), (3, "sh2"), (4, "sc2"), (5, "gt2")):
            wa2 = wa2b[mod - 2]
            wk = "wa2b%d" % (mod - 2)
            fw.dma("sp", "ld", lambda h, mod=mod: h.dma_start(out=btmp, in_=bada_d[0:1, mod * D:(mod + 1) * D].partition_broadcast(128)[:, 0, :]), writes=["btmp"])
            for half in range(2):
                pm = bank(half)
                for kc in range(8):
                    fw.op("pe", lambda h, kc=kc, half=half, pm=pm, wa2=wa2: h.matmul(pm, lhsT=Smat[:, kc, :], rhs=wa2[:, kc, half * 512:(half + 1) * 512], start=(kc == 0), stop=(kc == 7)), reads=["Smat", wk], writes=["pm%d" % half])
                hs = slice(half * 512, (half + 1) * 512)
                if nm == "sc2":
                    fw.op("dve", lambda h, hs=hs, pm=pm: h.tensor_tensor(out=btmp[:, hs], in0=pm, in1=btmp[:, hs], op=ALU.add), reads=["pm%d" % half, "btmp"], writes=["btmp"])
                    fw.op("dve", lambda h, hs=hs: h.scalar_tensor_tensor(out=bc["a2"][:, hs], in0=btmp[:, hs], scalar=1.0, in1=bc["a2"][:, hs], op0=ALU.add, op1=ALU.mult), reads=["btmp", "bc_a2"], writes=["bc_a2"])
                else:
                    fw.op("dve", lambda h, hs=hs, pm=pm, nm=nm: h.tensor_tensor(out=bc[nm][:, hs], in0=pm, in1=btmp[:, hs], op=ALU.add), reads=["pm%d" % half, "btmp"], writes=["bc_" + nm])
        fw.barrier(keep=["Wout"])

        o = 16384
        h2 = [carve(o + i * 4096, [1024], F32) for i in range(2)]; o += 8192
        tY = [carve(o + i * 4096, [1024], F32) for i in range(2)]; o += 8192
        h2T = carve(o, [8, 128], F32); o += 4096
        junkf = carve(o, [1024], BF16); o += 2048
        stat2 = carve(o, [NOWN, 8], F32); o += NOWN * 32
        assert o <= 40960
        h2ball = carve(O_POST, [NOWN, 1024], BF16)
        RBt = carve(146432, [12, NOWN, 32], F32)

        def opX(i):
            s = i % 2
            xlt = xlb[i % 3]
            xkey = "xlb%d" % (i % 3)
            fw.dma("sp", "ld", lambda h: h.dma_start(out=xlt, in_=x_d[i]), writes=[xkey])
            for half in range(2):
                for c in range(8):
                    fw.op("pe", lambda h, c=c, half=half: h.matmul(bank(half), lhsT=mixT[:, c, i * 128:(i + 1) * 128], rhs=Wout[:, c, half * 512:(half + 1) * 512], start=(c == 0), stop=(c == 7)),
                          reads=["mixT", "Wout"], writes=["pY%d" % half])
                fw.op("dve", lambda h, half=half: h.tensor_tensor(out=tY[s][:, half * 512:(half + 1) * 512], in0=bank(half), in1=bc["gt1"][:, half * 512:(half + 1) * 512], op=ALU.mult), reads=["pY%d" % half, "bc_gt1"], writes=["tY%d" % s])
            fw.op("dve", lambda h: h.tensor_tensor(out=xlt, in0=xlt, in1=tY[s], op=ALU.add), reads=[xkey, "tY%d" % s], writes=[xkey])
            fw.dma("sp", "st", lambda h: h.dma_start(out=xlscr[i], in_=xlt), reads=[xkey], writes=["xlscr"])
            if debug:
                fw.dma("sp", "dbg", lambda h: h.dma_start(out=dbg_xl[i], in_=xlt), reads=[xkey], writes=["dbg_xl"])
            fw.op("act", lambda h: h.activation(out=junkf, in_=xlt, func=AF.Square, accum_out=stat2[:, i, 0:1]), reads=[xkey], writes=["junkf", "st2_%d" % i])
            fw.op("dve", lambda h: h.tensor_scalar(out=stat2[:, i, 1:2], in0=stat2[:, i, 0:1], scalar1=1.0 / D, scalar2=EPS, op0=ALU.mult, op1=ALU.add), reads=["st2_%d" % i], writes=["st2_%d" % i])
            fw.op("act", lambda h: h.activation(out=stat2[:, i, 2:3], in_=stat2[:, i, 1:2], func=AF.Sqrt), reads=["st2_%d" % i], writes=["st2_%d" % i])
            fw.op("dve", lambda h: h.reciprocal(out=stat2[:, i, 3:4], in_=stat2[:, i, 2:3]), reads=["st2_%d" % i], writes=["st2_%d" % i])

        def opY(i):
            s = i % 2
            xlt = xlb[i % 3]
            xkey = "xlb%d" % (i % 3)
            fw.op("dve", lambda h: h.scalar_tensor_tensor(out=h2[s], in0=xlt, scalar=stat2[:, i, 3:4], in1=bc["a2"], op0=ALU.mult, op1=ALU.mult), reads=[xkey, "st2_%d" % i, "bc_a2"], writes=["h2_%d" % s])
            fw.op("dve", lambda h: h.tensor_tensor(out=h2[s], in0=h2[s], in1=bc["sh2"], op=ALU.add), reads=["h2_%d" % s, "bc_sh2"], writes=["h2_%d" % s])
            fw.op("act", lambda h: h.copy(out=h2ball[:, i, :], in_=h2[s]), reads=["h2_%d" % s], writes=["h2b%d" % i])
            for half in range(2):
                pv = PS[1][:, half, :].rearrange("p (c n) -> p c n", c=4)
                for c in range(4):
                    cc = half * 4 + c
                    fw.op("pe", lambda h, cc=cc, c=c, pv=pv: h.transpose(out=pv[:, c, :], in_=h2[s][:, cc * 128:(cc + 1) * 128], identity=identF[:]), reads=["h2_%d" % s, "identF"], writes=["pHT%d" % half])
                if half:
                    fw.op("act", lambda h, pv=pv: h.copy(out=h2T[:, 4:8, :], in_=pv), reads=["pHT1"], writes=["h2T"])
                else:
                    fw.op("dve", lambda h, pv=pv: h.tensor_copy(out=h2T[:, 0:4, :], in_=pv), reads=["pHT0"], writes=["h2T"])
            pl = PS[2][:, 0, 0:36]
            for c in range(8):
                fw.op("pe", lambda h, c=c: h.matmul(pl, lhsT=h2T[:, c, :], rhs=wr[:, c, :], start=(c == 0), stop=False), reads=["h2T", "wr"], writes=["pL"])
            fw.op("pe", lambda h: h.matmul(pl, lhsT=ones_f[0:1, :], rhs=brow[0:1, :], start=False, stop=True), reads=["ones_f", "brow"], writes=["pL"])
            fw.op("dve", lambda h: h.tensor_copy(out=Lall[:, i, :], in_=pl), reads=["pL"], writes=["Lall"])

        opX(0)
        for i in range(NOWN):
            if i + 1 < NOWN:
                opX(i + 1)
            opY(i)

        def RB(k, w=32):
            return RBt[:, k, :, 0:w]

        def bcl(ap2, w):
            return ap2.unsqueeze(2).to_broadcast([128, NOWN, w])

        def V(fn, reads=("rb",), writes=("rb",), eng="dve"):
            fw.op(eng, fn, reads=list(reads), writes=list(writes))

        G = Lall[:, :, 0:4]
        E4 = Lall[:, :, 4:36].rearrange("p i (g j) -> p i g j", g=4)
        c1 = lambda k: RBt[:, 11, :, k]
        V(lambda h: h.tensor_reduce(out=c1(0), in_=G, axis=AX.X, op=ALU.max), reads=("Lall", "rb"))
        V(lambda h: h.tensor_tensor(out=RB(0, 4), in0=G, in1=bcl(c1(0), 4), op=ALU.is_equal), reads=("Lall", "rb"))
        V(lambda h: h.tensor_tensor(out=RB(1, 4), in0=G, in1=bcl(c1(0), 4), op=ALU.subtract), reads=("Lall", "rb"))
        V(lambda h: h.activation(out=RB(2, 4), in_=RB(1, 4), func=AF.Exp), eng="act")
        V(lambda h: h.tensor_reduce(out=c1(1), in_=RB(2, 4), axis=AX.X, op=ALU.add))
        V(lambda h: h.reciprocal(out=c1(2), in_=c1(1)))
        V(lambda h: h.tensor_scalar(out=RB(3, 4), in0=RB(0, 4), scalar1=1e9, scalar2=-1e9, op0=ALU.mult, op1=ALU.add))
        M4 = RB(4).rearrange("p i (g j) -> p i g j", g=4)
        V(lambda h: h.tensor_tensor(out=M4, in0=E4, in1=RB(3, 4).unsqueeze(3).to_broadcast([128, NOWN, 4, 8]), op=ALU.add), reads=("Lall", "rb"))
        V(lambda h: h.tensor_reduce(out=c1(3), in_=RB(4), axis=AX.X, op=ALU.max))
        V(lambda h: h.tensor_tensor(out=RB(5), in0=RB(4), in1=bcl(c1(3), 32), op=ALU.is_equal))
        V(lambda h: h.scalar_tensor_tensor(out=RB(6), in0=RB(5), scalar=-1e9, in1=RB(4), op0=ALU.mult, op1=ALU.add))
        V(lambda h: h.tensor_reduce(out=c1(4), in_=RB(6), axis=AX.X, op=ALU.max))
        V(lambda h: h.tensor_tensor(out=RB(7), in0=RB(6), in1=bcl(c1(4), 32), op=ALU.is_equal))
        V(lambda h: h.tensor_tensor(out=c1(5), in0=c1(4), in1=c1(3), op=ALU.subtract))
        V(lambda h: h.activation(out=c1(6), in_=c1(5), func=AF.Exp), eng="act")
        V(lambda h: h.tensor_scalar(out=c1(7), in0=c1(6), scalar1=1.0, scalar2=0.0, op0=ALU.add, op1=ALU.add))
        V(lambda h: h.reciprocal(out=c1(8), in_=c1(7)))
        V(lambda h: h.tensor_tensor(out=gates[:, :, 0], in0=c1(8), in1=c1(2), op=ALU.mult), writes=("rb", "gates"))
        V(lambda h: h.tensor_tensor(out=gates[:, :, 1], in0=gates[:, :, 0], in1=c1(6), op=ALU.mult), reads=("rb", "gates"), writes=("rb", "gates"))
        V(lambda h: h.tensor_tensor(out=Bt[:], in0=RB(5), in1=RB(7), op=ALU.add), writes=("rb", "Bt"))
        pp = PS[2][:, 1, :].rearrange("p (i e) -> p i e", i=NOWN)
        for i in range(NOWN):
            for i2 in range(i + 1):
                fw.op("pe", lambda h, i2=i2, i=i: h.matmul(pp[:, i, :], lhsT=(lstB[:] if i2 == i else ones_b[:]), rhs=Bt[:, i2, :], start=(i2 == 0), stop=(i2 == i), skip_group_check=True), reads=["Bt", "lstB", "ones_b"], writes=["pP"])
        V(lambda h: h.tensor_copy(out=RB(8), in_=pp), reads=("pP", "rb"))
        iob = iotaF[:].unsqueeze(1).to_broadcast([128, NOWN, 32])
        for k, ohk in ((0, 5), (1, 7)):
            V(lambda h, ohk=ohk: h.tensor_tensor(out=RB(9), in0=RB(ohk), in1=RB(8), op=ALU.mult))
            V(lambda h: h.tensor_reduce(out=c1(9), in_=RB(9), axis=AX.X, op=ALU.add))
            V(lambda h, ohk=ohk: h.tensor_tensor(out=RB(9), in0=RB(ohk), in1=iob, op=ALU.mult), reads=("rb", "iotaF"))
            V(lambda h: h.tensor_reduce(out=c1(10), in_=RB(9), axis=AX.X, op=ALU.add))
            V(lambda h: h.tensor_scalar(out=c1(11), in0=c1(9), scalar1=float(CAP), scalar2=1e6, op0=ALU.is_ge, op1=ALU.mult))
            V(lambda h: h.scalar_tensor_tensor(out=c1(12), in0=c1(10), scalar=float(CAP), in1=c1(9), op0=ALU.mult, op1=ALU.add))
            V(lambda h: h.tensor_tensor(out=c1(13), in0=c1(12), in1=c1(11), op=ALU.add))
            V(lambda h: h.tensor_scalar(out=c1(14), in0=c1(13), scalar1=float(NSLOT), scalar2=0.0, op0=ALU.min, op1=ALU.add))
            V(lambda h, k=k: h.tensor_copy(out=desti[:, :, k, 0], in_=c1(13)), writes=("rb", "desti"))
            V(lambda h, k=k: h.tensor_copy(out=desti[:, :, k, 1], in_=c1(14)), writes=("rb", "desti"))
        for i in range(NOWN):
            for k in range(2):
                fw.dma("pool", "ind", lambda h, i=i, k=k: h.indirect_dma_start(out=xslots, out_offset=bass.IndirectOffsetOnAxis(ap=desti[:, i, k, 0:1], axis=0), in_=h2ball[:, i, :], in_offset=None, bounds_check=NSLOT - 1, oob_is_err=False),
                       reads=["h2b%d" % i, "desti"], writes=["xslots"])
        if debug:
            fw.dma("sp", "dbg", lambda h: h.dma_start(out=dbg_gates, in_=gates[:]), reads=["gates"], writes=["dbg_gates"])
            fw.dma("sp", "dbg", lambda h: h.dma_start(out=dbg_dest, in_=desti[:]), reads=["desti"], writes=["dbg_dest"])
        fw.barrier()

        NW = 3
        wbase = [0, 61440, 61440 + 24576]
        Wg = [carve(wbase[k], [8, 512], BF16) for k in range(NW)]
        Wu = [carve(wbase[k] + 8192, [8, 512], BF16) for k in range(NW)]
        Wd = [carve(wbase[k] + 16384, [4, 1024], BF16) for k in range(NW)]
        o = 24576
        xbT = [carve(o + i * 2048, [8, 128], BF16) for i in range(2)]; o += 4096
        sg = [carve(o + i * 2048, [512], F32) for i in range(2)]; o += 4096
        hm = [carve(o + i * 1024, [512], BF16) for i in range(2)]; o += 2048
        hmT = [carve(o + i * 1024, [4, 128], BF16) for i in range(2)]; o += 2048
        assert o <= 40960, o
        o = 61440 + 2 * 24576
        NXB, PF = 6, 4
        YG0 = o
        xb = [carve(o + i * 2048, [1024], BF16) for i in range(NXB)]; o += NXB * 2048
        NYS = 4
        ysb = [carve(o + i * 4096, [1024], F32) for i in range(NYS)]; o += NYS * 4096
        y01 = [carve(o + i * 4096, [1024], F32) for i in range(2)]; o += 8192
        xlf = [carve(o + i * 4096, [1024], F32) for i in range(2)]; o += 8192
        assert o <= USZ, o
        fw.op("dve", lambda h: h.memset(ysb[1][0:1, :], 0.0), writes=["ysb1"])
        fw.dma("sp", "st", lambda h: h.dma_start(out=ys[NSLOT:NSLOT + 1, :], in_=ysb[1][0:1, :]), reads=["ysb1"], writes=["ys"])
        NBLK = CAP // 128
        NB = 32 * NBLK

        def load_w(e, which):
            ws = e % NW
            if which == 0:
                fw.dma("pool", "wcast", lambda h: h.dma_start(out=Wg[ws].rearrange("p (a b) f -> p a (b f)", a=2), in_=wg_d[e].rearrange("(p a b) f -> p a (b f)", a=2, b=4)), writes=["Wg%d" % ws])
            elif which == 1:
                fw.dma("pool", "wcast", lambda h: h.dma_start(out=Wu[ws].rearrange("p (a b) f -> p a (b f)", a=2), in_=wu_d[e].rearrange("(p a b) f -> p a (b f)", a=2, b=4)), writes=["Wu%d" % ws])
            else:
                fw.dma("pool", "wcast", lambda h: h.dma_start(out=Wd[ws].rearrange("p (a b) f -> p a (b f)", a=2), in_=wd_d[e].rearrange("(p a b) f -> p a (b f)", a=2, b=2)), writes=["Wd%d" % ws])

        def load_xb(blk):
            e, b_ = blk // NBLK, blk % NBLK
            r0 = e * CAP + b_ * 128
            k = blk % NXB
            fw.dma("sp", "ld", lambda h: h.dma_start(out=xb[k], in_=xslots[r0:r0 + 128, :]), reads=["xslots"], writes=["xb%d" % k])

        def mA(blk):
            e, b_ = blk // NBLK, blk % NBLK
            ws = e % NW
            s_ = blk % 2
            k = blk % NXB
            if e + 2 < 32 and b_ < 3:
                load_w(e + 2, b_)
            if blk + PF < NB:
                load_xb(blk + PF)
            pT = bank_bf(4).rearrange("p (c n) -> p c n", c=8)
            for c in range(8):
                fw.op("pe", lambda h, c=c: h.transpose(out=pT[:, c, :], in_=xb[k].rearrange("p (n c) -> p c n", c=8)[:, c, :], identity=identB[:]), reads=["xb%d" % k, "identB"], writes=["b4"])
            fw.op("dve", lambda h: h.tensor_copy(out=xbT[s_], in_=pT), reads=["b4"], writes=["xbT%d" % s_])
            for c in range(8):
                fw.op("pe", lambda h, c=c: h.matmul(bank(2 * s_), lhsT=xbT[s_][:, c, :], rhs=Wg[ws][:, c, :], start=(c == 0), stop=(c == 7)), reads=["xbT%d" % s_, "Wg%d" % ws], writes=["pG%d" % s_])
            for c in range(8):
                fw.op("pe", lambda h, c=c: h.matmul(bank(2 * s_ + 1), lhsT=xbT[s_][:, c, :], rhs=Wu[ws][:, c, :], start=(c == 0), stop=(c == 7)), reads=["xbT%d" % s_, "Wu%d" % ws], writes=["pU%d" % s_])

        def mB(blk):
            e, b_ = blk // NBLK, blk % NBLK
            ws = e % NW
            s_ = blk % 2
            ky = blk % NYS
            r0 = e * CAP + b_ * 128
            fw.op("act", lambda h: h.activation(out=sg[s_], in_=bank(2 * s_), func=AF.Silu), reads=["pG%d" % s_], writes=["sg%d" % s_])
            fw.op("dve", lambda h: h.tensor_tensor(out=hm[s_], in0=sg[s_], in1=bank(2 * s_ + 1), op=ALU.mult), reads=["sg%d" % s_, "pU%d" % s_], writes=["hm%d" % s_])
            pH = bank_bf(5).rearrange("p (c n) -> p c n", c=8)
            for c in range(4):
                fw.op("pe", lambda h, c=c: h.transpose(out=pH[:, c, :], in_=hm[s_].rearrange("p (n c) -> p c n", c=4)[:, c, :], identity=identB[:]), reads=["hm%d" % s_, "identB"], writes=["b5"])
            fw.op("act", lambda h: h.copy(out=hmT[s_], in_=pH[:, 0:4, :]), reads=["b5"], writes=["hmT%d" % s_])
            for half in range(2):
                for c in range(4):
                    fw.op("pe", lambda h, c=c, half=half: h.matmul(PS[3][:, half, :], lhsT=hmT[s_][:, c, :], rhs=Wd[ws][:, c, half * 512:(half + 1) * 512], start=(c == 0), stop=(c == 3)), reads=["hmT%d" % s_, "Wd%d" % ws], writes=["pD%d" % half])
            fw.op("dve", lambda h: h.tensor_copy(out=ysb[ky][:, 0:512], in_=PS[3][:, 0, :]), reads=["pD0"], writes=["ysb%d" % ky])
            fw.op("act", lambda h: h.copy(out=ysb[ky][:, 512:1024], in_=PS[3][:, 1, :]), reads=["pD1"], writes=["ysb%d" % ky])
            fw.dma("sp", "st", lambda h: h.dma_start(out=ys[r0:r0 + 128, :], in_=ysb[ky]), reads=["ysb%d" % ky], writes=["ys"])

        for w in range(3):
            load_w(0, w)
        for w in range(3):
            load_w(1, w)
        for blk in range(PF):
            load_xb(blk)
        for step in range(NB + 1):
            if step < NB:
                mA(step)
            if step >= 1:
                mB(step - 1)
        fw.barrier()

        outs = []
        NY = 3
        yg = [[carve(YG0 + (r * 2 + k) * 4096, [1024], F32) for k in range(2)] for r in range(NY)]
        xlf3 = [carve(YG0 + NY * 8192 + r * 4096, [1024], F32) for r in range(3)]

        def fin_load(i):
            r = i % NY
            for k in range(2):
                fw.dma("pool", "ind", lambda h, k=k: h.indirect_dma_start(out=yg[r][k], out_offset=None, in_=ys, in_offset=bass.IndirectOffsetOnAxis(ap=desti[:, i, k, 1:2], axis=0)),
                       reads=["ys", "desti"], writes=["yg%d_%d" % (r, k)])
            fw.dma("sp", "ld", lambda h: h.dma_start(out=xlf3[i % 3], in_=xlscr[i]), reads=["xlscr"], writes=["xlf%d" % (i % 3)])

        def fin_comp(i):
            r = i % NY
            y0, y1 = yg[r]
            k0, k1 = "yg%d_0" % r, "yg%d_1" % r
            xf = xlf3[i % 3]
            xfk = "xlf%d" % (i % 3)
            fw.op("dve", lambda h: h.tensor_scalar(out=y0, in0=y0, scalar1=gates[:, i, 0:1], scalar2=0.0, op0=ALU.mult, op1=ALU.add), reads=[k0, "gates"], writes=[k0])
            fw.op("dve", lambda h: h.scalar_tensor_tensor(out=y0, in0=y1, scalar=gates[:, i, 1:2], in1=y0, op0=ALU.mult, op1=ALU.add), reads=[k0, k1, "gates"], writes=[k0])
            fw.op("pool", lambda h: h.tensor_tensor(out=y0, in0=y0, in1=bc["gt2"], op=ALU.mult), reads=[k0, "bc_gt2"], writes=[k0])
            fw.op("dve", lambda h: h.tensor_tensor(out=xf, in0=xf, in1=y0, op=ALU.add), reads=[k0, xfk], writes=[xfk])
            fw.op("act", lambda h: h.activation(out=junkf, in_=xf, func=AF.Square, accum_out=stat2[:, i, 4:5]), reads=[xfk], writes=["junkf", "st3_%d" % i])
            fw.op("dve", lambda h: h.tensor_scalar(out=stat2[:, i, 5:6], in0=stat2[:, i, 4:5], scalar1=1.0 / D, scalar2=EPS, op0=ALU.mult, op1=ALU.add), reads=["st3_%d" % i], writes=["st3_%d" % i])
            fw.op("act", lambda h: h.activation(out=stat2[:, i, 6:7], in_=stat2[:, i, 5:6], func=AF.Sqrt), reads=["st3_%d" % i], writes=["st3_%d" % i])
            fw.op("dve", lambda h: h.reciprocal(out=stat2[:, i, 7:8], in_=stat2[:, i, 6:7]), reads=["st3_%d" % i], writes=["st3_%d" % i])
            fw.op("act", lambda h: h.activation(out=xf, in_=xf, func=AF.Copy, scale=stat2[:, i, 7:8]), reads=[xfk, "st3_%d" % i], writes=[xfk])
            fw.op("pool", lambda h: h.tensor_tensor(out=xf, in0=xf, in1=bc["gfin"], op=ALU.mult), reads=[xfk, "bc_gfin"], writes=[xfk])
            fw.dma("sp", "out", lambda h: h.dma_start(out=y_d[i], in_=xf), reads=[xfk], writes=["y%d" % i])
            outs.append("y%d" % i)

        fin_load(0)
        fin_load(1)
        for i in range(NOWN):
            if i + 2 < NOWN:
                fin_load(i + 2)
            fin_comp(i)
        fw.final_wait("sp", outs)
        fw.emit(st)
    return nc


def _consts(j):
    f32 = np.float32
    bs = (16 * j + np.arange(NT)) % NT
    a = np.arange(128)
    inv_freq = 10000.0 ** (-np.arange(0, 32, 2, dtype=np.float64) / 32)
    rope = np.zeros((NT, 128, 128), f32)
    for u in range(NT):
        ang_r = a[:, None] * inv_freq[None, :]
        ang_c = np.full((128, 1), float(bs[u])) * inv_freq[None, :]
        ang = np.concatenate([ang_r, ang_r, ang_c, ang_c], axis=1)
        sgn = np.concatenate([-np.ones(16), np.ones(16), -np.ones(16), np.ones(16)])
        rope[u, :, 0:64] = np.cos(ang.astype(f32))
        rope[u, :, 64:128] = np.sin(ang.astype(f32)) * sgn
    cset = np.array([16 * j + i + 64 * h for h in range(2) for i in range(16)])
    n = 64 * a[:, None] + bs[None, :]
    th = 2 * np.pi * ((n[:, :, None] * cset[None, None, :]) % 8192) / 8192.0
    mb = np.concatenate([np.cos(th), -np.sin(th)], axis=2) / 32.0
    eb = np.zeros((3, 128, 128))
    d = np.arange(64)
    for h in range(2):
        for u in range(NT):
            ph = 2 * np.pi * ((bs[u] * d) % 64) / 64.0
            eb[0, h * 64 + u, 2 * d + h] = np.cos(ph) / 32.0
            eb[1, h * 64 + u, 2 * d + h] = np.sin(ph) / 32.0
    eb[2] = -eb[1]
    cc = np.arange(128)
    phc = 2 * np.pi * ((cc[:, None] * cc[None, :]) % 128) / 128.0
    cs = np.stack([np.cos(phc), np.sin(phc)])
    lst = (a[:, None] < a[None, :]).astype(f32)
    iota = np.tile(np.arange(32, dtype=f32)[None, :], (128, 1))
    return dict(rope=rope, mb=mb.astype(f32), eb=eb.astype(f32), cs=cs.astype(f32), lstrict=lst, iota=iota,
                ident=np.eye(128, dtype=f32))


_NC = None


def kernel(x, c, ctx, c_ctx, w_ada, b_ada, g_mix_norm, g_ffn_norm, w_in,
           lambda_q1, lambda_k1, lambda_q2, lambda_k2, g_subln, w_fourier, w_out,
           w_router_group, b_router_group, w_router_expert, b_router_expert,
           w_gate, w_up, w_down, g_final):
    global _NC
    if _NC is None:
        _NC = build_nc()
    nc = _NC
    in_maps = make_inputs(x, c, ctx, c_ctx, w_ada, b_ada, g_mix_norm, g_ffn_norm, w_in,
                          lambda_q1, lambda_k1, lambda_q2, lambda_k2, g_subln, w_fourier, w_out,
                          w_router_group, b_router_group, w_router_expert, b_router_expert,
                          w_gate, w_up, w_down, g_final)
    res = run_bass_kernel_spmd(nc, in_maps, core_ids=list(range(8)))
    f32 = np.float32
    out = np.zeros((2, 128, NT, D), f32)
    for core in range(8):
        b, j = core // 4, core % 4
        y = res.results[core]["y"]
        out[b][:, 16 * j:16 * j + 16, :] = y.transpose(1, 0, 2)
    return out.reshape(2, 8192, D)


def make_inputs(x, c, ctx, c_ctx, w_ada, b_ada, g_mix_norm, g_ffn_norm, w_in,
                lambda_q1, lambda_k1, lambda_q2, lambda_k2, g_subln, w_fourier, w_out,
                w_router_group, b_router_group, w_router_expert, b_router_expert,
                w_gate, w_up, w_down, g_final):
    f32 = np.float32
    A = lambda v: np.ascontiguousarray(np.asarray(v, dtype=f32))
    x = A(x); c = A(c); ctx = A(ctx); c_ctx = A(c_ctx)
    shared = dict(
        w_ada=A(w_ada[0]), b_ada=A(b_ada[0]).reshape(1, -1),
        badaT=A(np.asarray(b_ada[0]).reshape(6, 8, 128).transpose(2, 0, 1)),
        gmixT=A(np.asarray(g_mix_norm[0]).reshape(8, 128).T),
        g_ffn=A(g_ffn_norm[0]).reshape(1, -1), g_final=A(g_final).reshape(1, -1),
        w_in=A(w_in[0]),
        lam4=A(np.concatenate([np.asarray(lambda_q1[0]), np.asarray(lambda_k1[0]), np.asarray(lambda_q2[0]), np.asarray(lambda_k2[0])])).reshape(1, 256),
        g_subln=A(g_subln[0]).reshape(1, 128), w_fourier=A(w_fourier[0]), w_out=A(w_out[0]),
        w_router=A(np.concatenate([np.asarray(w_router_group[0]), np.asarray(w_router_expert[0])], axis=1)),
        b_router=A(np.concatenate([np.asarray(b_router_group[0]), np.asarray(b_router_expert[0])])).reshape(1, 36),
        w_gate=A(w_gate[0]), w_up=A(w_up[0]), w_down=A(w_down[0]),
    )
    consts = [_consts(j) for j in range(4)]
    in_maps = []
    for core in range(8):
        b, j = core // 4, core % 4
        bs = (16 * j + np.arange(NT)) % NT
        xt = x[b].reshape(128, NT, D).transpose(1, 0, 2)[bs]
        cc = np.stack([c[b], c_ctx], axis=1)
        m = dict(shared)
        m.update(consts[j])
        m["x"] = np.ascontiguousarray(xt)
        m["ctx"] = np.ascontiguousarray(ctx[b].reshape(2, 128, D))
        m["ccT"] = np.ascontiguousarray(cc.reshape(8, 128, 2).transpose(1, 0, 2))
        in_maps.append(m)
    return in_maps
```

```python
import contextlib
import numpy as np
import concourse.bass as bass
import concourse.mybir as mybir
from concourse.bass_utils import run_bass_kernel_spmd

F32 = mybir.dt.float32
BF16 = mybir.dt.bfloat16
I32 = mybir.dt.int32
AF = mybir.ActivationFunctionType
ALU = mybir.AluOpType
AX = mybir.AxisListType

D = 1024
NT = 64
NOWN = 16
NKT = 66
CAP = 384
NSLOT = 32 * CAP
EPS = 1e-6
LAMBDA_INIT = 0.2


class FW:
    ENG = ("pe", "act", "dve", "pool", "sp")

    def __init__(self, nc):
        self.nc = nc
        self.ops = {e: [] for e in self.ENG}
        self.seq = {e: 0 for e in self.ENG}
        self.lastc = {}
        self.waited = {e: {} for e in self.ENG}
        self.dma_cnt = {}
        self.dma_rr = {}
        self.lastw = {}
        self.readers = {}
        self.needed = set()

    def _deps(self, eng, reads, writes):
        ev = []
        for r in reads:
            if r in self.lastw:
                ev.append(self.lastw[r])
        for w in writes:
            if w in self.lastw:
                ev.append(self.lastw[w])
            ev.extend(self.readers.get(w, ()))
        best = {}
        for (k, v) in ev:
            if k == eng and eng == "pe":
                continue
            if v > best.get(k, 0):
                best[k] = v
        return self._waits(eng, best)

    def _waits(self, eng, best):
        waits = []
        wd = self.waited[eng]
        for k, v in best.items():
            if wd.get(k, 0) >= v:
                continue
            wd[k] = v
            waits.append((k, v))
            if k in self.ENG:
                self.needed.add((k, v))
        return waits

    def _mark(self, token, reads, writes):
        for r in reads:
            self.readers.setdefault(r, []).append(token)
        for w in writes:
            self.lastw[w] = token
            self.readers[w] = []

    def op(self, eng, fn, reads=(), writes=()):
        waits = self._deps(eng, reads, writes)
        self.seq[eng] += 1
        tok = (eng, self.seq[eng])
        self.lastc[eng] = tok
        self.ops[eng].append([fn, waits, "c", tok])
        self._mark(tok, reads, writes)
        return tok

    NSEM = 20

    def dma(self, q, stream, fn, reads=(), writes=()):
        r = self.dma_rr.get(q, 0)
        self.dma_rr[q] = (r + 1) % self.NSEM
        key = "dq_%s_%d" % (q, r)
        prev = self.dma_cnt.get(key, 0)
        waits = self._deps(q, reads, writes)
        if prev:
            waits = waits + self._waits(q, {key: prev})
        self.dma_cnt[key] = prev + 16
        tok = (key, prev + 16)
        self.ops[q].append([fn, waits, "d", tok])
        self._mark(tok, reads, writes)
        return tok

    def barrier(self, keep=()):
        best = {k: v for (k, v) in self.lastc.values()}
        for k, v in self.dma_cnt.items():
            best[k] = v
        kept = {}
        for key in keep:
            if key in self.lastw:
                tk = self.lastw[key]
                kept[key] = tk
                if tk[0] in self.dma_cnt and best.get(tk[0], 0) == tk[1]:
                    best[tk[0]] = tk[1] - 16
        for e in self.ENG:
            b = {k: v for k, v in best.items() if k != e and v > 0}
            self.ops[e].append([None, self._waits(e, b), "w", None])
        self.lastw = dict(kept)
        self.readers = {k: [] for k in kept}

    def final_wait(self, eng, keys):
        waits = self._deps(eng, list(keys), [])
        self.ops[eng].append([None, waits, "w", None])

    def emit(self, stack):
        nc = self.nc
        sems = {}
        for e in self.ENG:
            sems[e] = stack.enter_context(nc.semaphore("s_" + e))
        for k in self.dma_cnt:
            sems[k] = stack.enter_context(nc.semaphore(k))
        sigval = {}
        for e in self.ENG:
            c = 0
            for o in self.ops[e]:
                if o[2] == "c" and o[3] in self.needed:
                    c += 1
                    sigval[o[3]] = c
        block = stack.enter_context(nc.Block())

        def run(e, h):
            for fn, waits, kind, tok in self.ops[e]:
                for (k, v) in waits:
                    h.wait_ge(sems[k], sigval[(k, v)] if k in self.ENG else v)
                if kind == "w":
                    continue
                ins = fn(h)
                if kind == "d":
                    ins.then_inc(sems[tok[0]], 16)
                elif tok in sigval:
                    ins.then_inc(sems[e], 1)

        @block.tensor
        def _(h):
            run("pe", h)

        @block.scalar
        def _(h):
            run("act", h)

        @block.vector
        def _(h):
            run("dve", h)

        @block.gpsimd
        def _(h):
            run("pool", h)

        @block.sync
        def _(h):
            run("sp", h)


def build_nc(debug=False):
    nc = bass.Bass("TRN2", target_bir_lowering=False)

    def din(name, shape, dt=F32):
        return nc.dram_tensor(name, list(shape), dt, kind="ExternalInput").ap()

    x_d = din("x", [NT, 128, D])
    ctx_d = din("ctx", [2, 128, D])
    ccT_d = din("ccT", [128, 8, 2])
    wada_d = din("w_ada", [D, 6 * D])
    badaT_d = din("badaT", [128, 6, 8])
    bada_d = din("b_ada", [1, 6 * D])
    gmixT_d = din("gmixT", [128, 8])
    gffn_d = din("g_ffn", [1, D])
    gfin_d = din("g_final", [1, D])
    win_d = din("w_in", [D, 2048])
    lam_d = din("lam4", [1, 256])
    gsub_d = din("g_subln", [1, 128])
    wf_d = din("w_fourier", [4, 128, 128])
    wout_d = din("w_out", [D, D])
    wr_d = din("w_router", [D, 36])
    br_d = din("b_router", [1, 36])
    wg_d = din("w_gate", [32, D, 512])
    wu_d = din("w_up", [32, D, 512])
    wd_d = din("w_down", [32, 512, D])
    ident_d = din("ident", [128, 128])
    rope_d = din("rope", [NT, 128, 128])
    mb_d = din("mb", [128, NT, 64])
    eb_d = din("eb", [3, 128, 128])
    cs_d = din("cs", [2, 128, 128])
    lst_d = din("lstrict", [128, 128])
    iota_d = din("iota", [128, 32])
    y_d = nc.dram_tensor("y", [NOWN, 128, D], F32, kind="ExternalOutput").ap()

    if debug:
        dbg_attn = nc.dram_tensor("dbg_attn", [128, NOWN, 512], F32, kind="ExternalOutput").ap()
        dbg_fo = nc.dram_tensor("dbg_fo", [128, 4, 2048], F32, kind="ExternalOutput").ap()
        dbg_xl = nc.dram_tensor("dbg_xl", [NOWN, 128, D], F32, kind="ExternalOutput").ap()
        dbg_gates = nc.dram_tensor("dbg_gates", [128, NOWN, 2], F32, kind="ExternalOutput").ap()
        dbg_dest = nc.dram_tensor("dbg_dest", [128, NOWN, 2, 2], I32, kind="ExternalOutput").ap()
        dbg_q = nc.dram_tensor("dbg_q", [128, 4, 2048], F32, kind="ExternalOutput").ap()
    vscr = nc.dram_tensor("vscr", [128, NKT, 4 * 129], BF16, kind="Internal").ap()
    zscr = nc.dram_tensor("zscr", [64, NT, 512], BF16, kind="Internal").ap()
    xslots = nc.dram_tensor("xslots", [NSLOT, D], BF16, kind="Internal").ap()
    ys = nc.dram_tensor("ys", [NSLOT + 1, D], F32, kind="Internal").ap()
    xlscr = nc.dram_tensor("xlscr", [NOWN, 128, D], F32, kind="Internal").ap()

    fw = FW(nc)
    with contextlib.ExitStack() as st:
        USZ = 168 * 1024
        U = st.enter_context(nc.sbuf_tensor("U", [128, USZ // 2], BF16))

        def carve(off, dims, dt, parts=128):
            esz = 4 if dt in (F32, I32) else 2
            n = int(np.prod(dims)) * esz
            assert off % 4 == 0 and off + n <= USZ, (off, n)
            ap = U[0:parts, off // 2:(off + n) // 2]
            if dt != BF16:
                ap = ap.bitcast(dt)
            if len(dims) > 1:
                names = " ".join("d%d" % i for i in range(len(dims)))
                kw = {"d%d" % i: dims[i] for i in range(len(dims))}
                ap = ap.rearrange("p (%s) -> p %s" % (names, names), **kw)
            return ap

        def T(name, shape, dt):
            return st.enter_context(nc.sbuf_tensor("sb_" + name, list(shape), dt))

        PS = [st.enter_context(nc.psum_tensor("ps%d" % i, [128, 2, 512], F32)) for i in range(4)]

        def bank(i):
            return PS[i // 2][:, i % 2, :]

        def bank_bf(i):
            return PS[i // 2][:, i % 2, :].bitcast(BF16)

        identF = T("identF", [128, 128], F32)
        identB = T("identB", [128, 128], BF16)
        ones_f = T("ones_f", [128, 128], F32)
        ones_b = T("ones_b", [128, 128], BF16)
        lstB = T("lstB", [128, 128], BF16)
        iotaF = T("iotaF", [128, 32], F32)
        ccT = T("ccT", [128, 8, 2], F32)
        scT = T("scT", [128, 8, 2], F32)
        badaT = T("badaT", [128, 6, 8], F32)
        gmixT = T("gmixT", [128, 8], F32)
        modT = T("modT", [128, 2, 8, 2], F32)
        amod = T("amod", [128, 2, 8], F32)
        smod = T("smod", [128, 2, 8], F32)
        stat = T("stat", [128, NKT, 4], F32)
        lamt = T("lamt", [128, 256], F32)
        lamw = T("lamw", [128, 8], F32)
        gsub = T("gsub", [128, 128], F32)
        attn_tok = T("attn_tok", [128, NOWN, 512], BF16)
        small = T("small", [128, 64], F32)
        AB = T("AB", [128, 2, 4, 128], BF16)
        EB = T("EB", [128, 3, 128], BF16)
        gates = T("gates", [128, NOWN, 2], F32)
        desti = T("desti", [128, NOWN, 2, 2], I32)
        Bt = T("Bt", [128, NOWN, 32], BF16)
        rt = T("rt", [128, 13, 36], F32)
        wr = T("wr", [128, 8, 36], F32)
        brow = T("brow", [1, 36], F32)

        dq = {"n": 0}

        def ld(out, in_, key, q="sp", stream="ld"):
            return fw.dma(q, stream, lambda h: h.dma_start(out=out, in_=in_), writes=[key])

        ld(identF[:], ident_d, "identF")
        ld(identB[:], ident_d, "identB", q="pool", stream="cast")
        ld(lstB[:], lst_d, "lstB", q="pool", stream="cast")
        ld(EB[:], eb_d.rearrange("e p n -> p e n"), "EB", q="pool", stream="cast")
        ld(iotaF[:], iota_d, "iotaF")
        ld(ccT[:], ccT_d, "ccT")
        ld(badaT[:], badaT_d, "badaT")
        ld(gmixT[:], gmixT_d, "gmixT")
        ld(lamt[:], lam_d.partition_broadcast(128)[:, 0, :], "lamt")
        ld(gsub[:], gsub_d.partition_broadcast(128)[:, 0, :], "gsub")
        ld(wr[:], wr_d.rearrange("(c p) n -> p c n", p=128), "wr")
        ld(brow[:], br_d, "brow")
        fw.op("dve", lambda h: h.memset(ones_f[:], 1.0), writes=["ones_f"])
        fw.op("dve", lambda h: h.memset(stat[:], 0.0), writes=["stat%d" % t for t in range(NKT)])
        fw.op("dve", lambda h: h.memset(small[:], 0.0), writes=["small"])
        fw.op("dve", lambda h: h.memset(lamw[:], 0.0), writes=["lamw"])
        fw.op("dve", lambda h: h.memset(ones_b[:], 1.0), writes=["ones_b"])
        fw.op("act", lambda h: h.activation(out=scT[:], in_=ccT[:], func=AF.Silu), reads=["ccT"], writes=["scT"])
        lt = lamt[:].rearrange("p (k d) -> p k d", k=4)
        fw.op("dve", lambda h: h.tensor_tensor(out=small[:, 0:64], in0=lt[:, 0, :], in1=lt[:, 1, :], op=ALU.mult), reads=["lamt"], writes=["small"])
        fw.op("dve", lambda h: h.reduce_sum(out=lamw[:, 0:1], in_=small[:, 0:64], axis=AX.X), reads=["small"], writes=["lamw"])
        fw.op("dve", lambda h: h.tensor_tensor(out=small[:, 0:64], in0=lt[:, 2, :], in1=lt[:, 3, :], op=ALU.mult), reads=["lamt", "lamw"], writes=["small"])
        fw.op("dve", lambda h: h.reduce_sum(out=lamw[:, 1:2], in_=small[:, 0:64], axis=AX.X), reads=["small"], writes=["lamw"])
        fw.op("act", lambda h: h.activation(out=lamw[:, 2:4], in_=lamw[:, 0:2], func=AF.Exp), reads=["lamw"], writes=["lamw"])
        fw.op("dve", lambda h: h.tensor_tensor(out=lamw[:, 4:5], in0=lamw[:, 3:4], in1=lamw[:, 2:3], op=ALU.subtract), reads=["lamw"], writes=["lamw"])
        fw.op("dve", lambda h: h.tensor_scalar(out=lamw[:, 4:5], in0=lamw[:, 4:5], scalar1=-LAMBDA_INIT, scalar2=0.0, op0=ALU.add, op1=ALU.add), reads=["lamw"], writes=["lamw"])
        fw.op("dve", lambda h: h.tensor_scalar(out=gsub[:], in0=gsub[:], scalar1=1.0 - LAMBDA_INIT, scalar2=0.0, op0=ALU.mult, op1=ALU.add), reads=["gsub"], writes=["gsub"])

        O_POST_C = 101376
        O_KT = 0
        O_QT = O_KT + 4 * 8448 * 2
        O_WIN = O_QT + 4 * 2048 * 2
        O_W = O_WIN + 8 * 2048 * 2
        KT = carve(O_KT, [4, 8448], BF16)
        QT = carve(O_QT, [4, 2048], BF16)
        Win = carve(O_WIN, [8, 2048], BF16)
        o = O_W
        Mb = carve(o, [NT, 64], BF16); o += NT * 64 * 2
        wa = carve(o, [8, 1024], F32)
        xt = [carve(o + i * 4096, [1024], F32) for i in range(2)]; o += 8192
        xn = [carve(o + i * 2048, [1024], BF16) for i in range(2)]; o += 4096
        hlT = [carve(o + i * 2048, [8, 128], BF16) for i in range(2)]; o += 4096
        rp = [carve(o + i * 512, [128], F32) for i in range(3)]; o += 1536
        tcs = carve(o, [2, 512], F32); o += 4096
        ktok = [carve(o + i * 1024, [512], BF16) for i in range(2)]; o += 2048
        qtok = [carve(o + i * 1024, [512], BF16) for i in range(2)]; o += 2048
        vst = [carve(o + i * 1032, [4, 129], BF16) for i in range(2)]; o += 2064
        fb = [carve(o + i * 1024, [512], BF16) for i in range(2)]; o += 2048
        zst = [carve(o + i * 4096, [4, 512], BF16, parts=64) for i in range(2)]; o += 8192
        junk = carve(o, [1024], BF16); o += 2048
        assert o <= USZ, o

        for c in range(8):
            fw.dma("pool", "cast", lambda h, c=c: h.dma_start(out=Win[:, c, :], in_=win_d[c * 128:(c + 1) * 128, :]), writes=["Win"])
        ld(Mb, mb_d, "Mb", q="pool", stream="cast")

        for mod in range(2):
            for c in range(8):
                fw.dma("sp", "ld", lambda h, c=c, mod=mod: h.dma_start(out=wa[:, c, :], in_=wada_d[c * 128:(c + 1) * 128, mod * D:(mod + 1) * D]), writes=["wa"])
            pm = bank(7)
            for mc in range(8):
                for kc in range(8):
                    fw.op("pe", lambda h, mc=mc, kc=kc: h.matmul(pm[:, mc * 2:mc * 2 + 2], lhsT=wa[:, kc, mc * 128:(mc + 1) * 128], rhs=scT[:, kc, :], start=(kc == 0), stop=(kc == 7)),
                          reads=["wa", "scT"], writes=["b7"])
            fw.op("dve", lambda h, mod=mod: h.tensor_copy(out=modT[:, mod, :, :], in_=pm[:, 0:16].rearrange("p (a b) -> p a b", b=2)), reads=["b7"], writes=["modT"])
        for mod in range(2):
            for j in range(2):
                fw.op("dve", lambda h, mod=mod, j=j: h.tensor_tensor(out=modT[:, mod, :, j], in0=modT[:, mod, :, j], in1=badaT[:, mod, :], op=ALU.add), reads=["modT", "badaT"], writes=["modT"])
        for j in range(2):
            fw.op("dve", lambda h, j=j: h.scalar_tensor_tensor(out=amod[:, j, :], in0=modT[:, 1, :, j], scalar=1.0, in1=gmixT[:], op0=ALU.add, op1=ALU.mult), reads=["modT", "gmixT"], writes=["amod"])
            fw.op("dve", lambda h, j=j: h.tensor_copy(out=smod[:, j, :], in_=modT[:, 0, :, j]), reads=["modT"], writes=["smod"])

        fw.barrier()
        for i in range(2):
            fw.op("dve", lambda h, i=i: h.memset(vst[i][:, :, 128:129], 1.0), writes=["vst%d" % i])
        B_T, B_K, B_Q, B_V, B_F, B_Z, B_X = 0, 1, 2, 3, 4, 5, 6

        def tinfo(t):
            lat = t >= 2
            u = t - 2
            return lat, u, (lat and u < NOWN), t % 2

        def st1(t):
            lat, u, own, s = tinfo(t)
            src = x_d[u] if lat else ctx_d[t]
            fw.dma("sp", "ld", lambda h: h.dma_start(out=xt[s], in_=src), writes=["xt%d" % s])
            if lat:
                fw.dma("sp", "ld", lambda h: h.dma_start(out=rp[t % 3], in_=rope_d[u]), writes=["rp%d" % (t % 3)])
            fw.op("act", lambda h: h.activation(out=junk, in_=xt[s], func=AF.Square, accum_out=stat[:, t, 0:1]), reads=["xt%d" % s], writes=["junk", "stat%d" % t])
            fw.op("dve", lambda h: h.tensor_scalar(out=stat[:, t, 1:2], in0=stat[:, t, 0:1], scalar1=1.0 / D, scalar2=EPS, op0=ALU.mult, op1=ALU.add), reads=["stat%d" % t], writes=["stat%d" % t])
            fw.op("act", lambda h: h.activation(out=stat[:, t, 2:3], in_=stat[:, t, 1:2], func=AF.Sqrt), reads=["stat%d" % t], writes=["stat%d" % t])
            fw.op("dve", lambda h: h.reciprocal(out=stat[:, t, 3:4], in_=stat[:, t, 2:3]), reads=["stat%d" % t], writes=["stat%d" % t])
            fw.op("act", lambda h: h.activation(out=xn[s], in_=xt[s], func=AF.Copy, scale=stat[:, t, 3:4]), reads=["xt%d" % s, "stat%d" % t], writes=["xn%d" % s])

        def st1b(t):
            lat, u, own, s = tinfo(t)
            pT = bank_bf(B_T).rearrange("p (c n) -> p c n", c=8)
            for c in range(8):
                fw.op("pe", lambda h, c=c: h.transpose(out=pT[:, c, :], in_=xn[s][:, c * 128:(c + 1) * 128], identity=identB[:]), reads=["xn%d" % s, "identB"], writes=["bT"])
            jj = 0 if lat else 1
            for c in range(8):
                fw.op("dve", lambda h, c=c: h.tensor_scalar(out=hlT[s][:, c, :], in0=pT[:, c, :], scalar1=amod[:, jj, c:c + 1], scalar2=smod[:, jj, c:c + 1], op0=ALU.mult, op1=ALU.add),
                      reads=["bT", "amod", "smod"], writes=["hlT%d" % s])

        def rope(bk, key, dst, dkey, r):
            s = r
            xv = bank(bk)
            x3 = xv.rearrange("p (g d) -> p g d", g=8)
            cosb = rp[s][:, 0:64].unsqueeze(1).to_broadcast([128, 8, 64])
            sinv = rp[s][:, 64:128].rearrange("p (a t j) -> p a t j", a=2, t=2)
            x5 = xv.rearrange("p (g a t j) -> p g a t j", g=8, a=2, t=2)
            tc3 = tcs[:, 0, :].rearrange("p (g d) -> p g d", g=8)
            ts5 = tcs[:, 1, :].rearrange("p (g a t j) -> p g a t j", g=8, a=2, t=2)
            fw.op("dve", lambda h: h.tensor_tensor(out=tc3, in0=x3, in1=cosb, op=ALU.mult), reads=[key, "rp%d" % s], writes=["tcs0"])
            for tt in range(2):
                sb_ = sinv[:, :, tt, :].unsqueeze(1).to_broadcast([128, 8, 2, 16])
                fw.op("dve", lambda h, tt=tt, sb_=sb_: h.tensor_tensor(out=ts5[:, :, :, tt, :], in0=x5[:, :, :, 1 - tt, :], in1=sb_, op=ALU.mult), reads=[key, "rp%d" % s], writes=["tcs1"])
            fw.op("dve", lambda h: h.tensor_tensor(out=dst, in0=tcs[:, 0, :], in1=tcs[:, 1, :], op=ALU.add), reads=["tcs0", "tcs1"], writes=[dkey])

        def st2(t):
            lat, u, own, s = tinfo(t)
            blocks = [(B_K, 512, "bK"), (B_V, 1024, "bV")]
            if lat:
                blocks.append((B_F, 1536, "bF"))
            if own:
                blocks.append((B_Q, 0, "bQ"))
            for (bk, col, key) in blocks:
                for c in range(8):
                    fw.op("pe", lambda h, c=c, bk=bk, col=col: h.matmul(bank(bk), lhsT=hlT[s][:, c, :], rhs=Win[:, c, col:col + 512], start=(c == 0), stop=(c == 7)),
                          reads=["hlT%d" % s, "Win"], writes=[key])
            if lat:
                rope(B_K, "bK", ktok[s], "ktok%d" % s, t % 3)
            else:
                fw.op("dve", lambda h: h.tensor_copy(out=ktok[s], in_=bank(B_K)), reads=["bK"], writes=["ktok%d" % s])
            if own:
                rope(B_Q, "bQ", qtok[s], "qtok%d" % s, t % 3)
            fw.op("act", lambda h: h.copy(out=vst[s][:, :, 0:128], in_=bank(B_V).rearrange("p (g d) -> p g d", g=4)), reads=["bV"], writes=["vst%d" % s])
            fw.dma("sp", "st", lambda h: h.dma_start(out=vscr[:, t, :], in_=vst[s].rearrange("p g d -> p (g d)")), reads=["vst%d" % s], writes=["vscr"])
            if lat:
                fw.op("act", lambda h: h.copy(out=fb[s], in_=bank(B_F)), reads=["bF"], writes=["fb%d" % s])

        def st3(t):
            lat, u, own, s = tinfo(t)
            pX = bank_bf(B_X).rearrange("p (c n) -> p c n", c=8)
            for hh in range(4):
                fw.op("pe", lambda h, hh=hh: h.transpose(out=pX[:, hh, :], in_=ktok[s][:, hh * 128:(hh + 1) * 128], identity=identB[:]), reads=["ktok%d" % s, "identB"], writes=["bX"])
            if own:
                for hh in range(4):
                    fw.op("pe", lambda h, hh=hh: h.transpose(out=pX[:, 4 + hh, :], in_=qtok[s][:, hh * 128:(hh + 1) * 128], identity=identB[:]), reads=["qtok%d" % s, "identB"], writes=["bX"])
            fw.op("act", lambda h: h.copy(out=KT[:, :, t * 128:(t + 1) * 128], in_=pX[:, 0:4, :]), reads=["bX"], writes=["KT"])
            if own:
                fw.op("act", lambda h: h.copy(out=QT[:, :, u * 128:(u + 1) * 128], in_=pX[:, 4:8, :]), reads=["bX"], writes=["QT"])
            if lat:
                fw.op("pe", lambda h: h.matmul(bank(B_Z)[0:64, :], lhsT=Mb[:, u, :], rhs=fb[s], start=True, stop=True), reads=["fb%d" % s, "Mb"], writes=["bZ"])
                zs = (u // 4) % 2
                fw.op("dve", lambda h: h.tensor_copy(out=zst[zs][:, u % 4, :], in_=bank(B_Z)[0:64, :]), reads=["bZ"], writes=["zst%d" % zs])
                if u % 4 == 3:
                    fw.dma("sp", "st", lambda h: h.dma_start(out=zscr[:, u - 3:u + 1, :], in_=zst[zs]), reads=["zst%d" % zs], writes=["zscr"])

        st1(0)
        for step in range(NKT + 2):
            if step + 1 < NKT:
                st1(step + 1)
            if step < NKT:
                st1b(step)
            if 1 <= step <= NKT:
                st2(step - 1)
            if step >= 2:
                st3(step - 2)

        fw.barrier()
        if debug:
            fw.dma("pool", "dbg", lambda h: h.dma_start(out=dbg_q, in_=QT), reads=["QT"], writes=["dbg_q"])
        O_VA = O_WIN
        o = O_VA
        VA = carve(o, [NKT, 4, 129], BF16); o += NKT * 4 * 129 * 2
        o = (o + 3) // 4 * 4
        ET = [carve(o + i * 2048, [2, 512], BF16) for i in range(2)]; o += 4096
        qp = [[carve(o + (i * 2 + m) * 1024, [512], BF16) for m in range(2)] for i in range(2)]; o += 4096
        osb = carve(o, [4, 128], F32); o += 2048
        osq = carve(o, [128], F32); o += 512
        Ocp = carve(o, [4, 512], F32); o += 8192
        assert o <= USZ, o
        for g in range(6):
            fw.dma("sp", "ld", lambda h, g=g: h.dma_start(out=VA[:, g * 11:(g + 1) * 11, :, :].rearrange("p t g d -> p t (g d)"), in_=vscr[:, g * 11:(g + 1) * 11, :]), reads=["vscr"], writes=["VA"])
        for i in range(2):
            for m in range(2):
                fw.op("pool", lambda h, i=i, m=m: h.memset(qp[i][m], 0.0), writes=["qp%d" % i])
        SB = [PS[0], PS[1]]
        OB = [PS[2], PS[3]]

        def Oap(m, j):
            return OB[m][:, j // 2, (j % 2) * 256:(j % 2) * 256 + 129]

        iters = [(qb, hh, kt) for qb in range(4) for hh in range(4) for kt in range(NKT)]

        def emit_qp(qb, hh):
            pass

        def emit_S(n):
            qb, hh, kt = iters[n]
            sb = n % 2
            for m in range(2):
                fw.op("pe", lambda h, m=m: h.matmul(SB[sb][:, m, :], lhsT=KT[m * 64:(m + 1) * 64, hh, kt * 128:(kt + 1) * 128], rhs=QT[m * 64:(m + 1) * 64, hh, qb * 512:(qb + 1) * 512], start=True, stop=True, tile_position=(m * 64, 0)),
                      reads=["KT", "QT"], writes=["S%d" % sb])

        def emit_exp(n):
            sb = n % 2
            fw.op("act", lambda h: h.activation(out=ET[sb], in_=SB[sb][:, :, :], func=AF.Exp, scale=0.125), reads=["S%d" % sb], writes=["ET%d" % sb])

        def emit_AV(n):
            qb, hh, kt = iters[n]
            sb = n % 2
            for m in range(2):
                for j in range(4):
                    fw.op("pe", lambda h, m=m, j=j: h.matmul(Oap(m, j), lhsT=ET[sb][:, m, j * 128:(j + 1) * 128], rhs=VA[:, kt, hh, :], start=(kt == 0 and j % 2 == 0), stop=(kt == NKT - 1), skip_group_check=True),
                          reads=["ET%d" % sb, "VA"], writes=["O"])

        def Ocp_ap(m, j):
            return Ocp[:, m * 2 + j // 2, (j % 2) * 256:(j % 2) * 256 + 129]

        def emit_norm1(qb, hh):
            for bk in range(4):
                fw.op("dve", lambda h, bk=bk: h.tensor_copy(out=Ocp[:, bk, :], in_=OB[bk // 2][:, bk % 2, :]), reads=["O"], writes=["Ocp"])
            for j in range(4):
                fw.op("dve", lambda h, j=j: h.reciprocal(out=small[:, 0:1], in_=Ocp_ap(0, j)[:, 128:129]), reads=["Ocp"], writes=["small"])
                fw.op("dve", lambda h, j=j: h.reciprocal(out=small[:, 1:2], in_=Ocp_ap(1, j)[:, 128:129]), reads=["Ocp"], writes=["small"])
                fw.op("dve", lambda h: h.tensor_tensor(out=small[:, 2:3], in0=small[:, 1:2], in1=lamw[:, 4:5], op=ALU.mult), reads=["small", "lamw"], writes=["small"])
                fw.op("dve", lambda h, j=j: h.tensor_scalar(out=osb[:, j, :], in0=Ocp_ap(0, j)[:, 0:128], scalar1=small[:, 0:1], scalar2=0.0, op0=ALU.mult, op1=ALU.add), reads=["Ocp", "small"], writes=["osb"])
                fw.op("dve", lambda h, j=j: h.scalar_tensor_tensor(out=osb[:, j, :], in0=Ocp_ap(1, j)[:, 0:128], scalar=small[:, 2:3], in1=osb[:, j, :], op0=ALU.mult, op1=ALU.add), reads=["Ocp", "small", "osb"], writes=["osb"])
                fw.op("dve", lambda h, j=j: h.tensor_tensor(out=osq, in0=osb[:, j, :], in1=osb[:, j, :], op=ALU.mult), reads=["osb"], writes=["osq"])
                fw.op("dve", lambda h, j=j: h.reduce_sum(out=small[:, 8 + j:9 + j], in_=osq, axis=AX.X), reads=["osq"], writes=["small"])
            fw.op("dve", lambda h: h.tensor_scalar(out=small[:, 12:16], in0=small[:, 8:12], scalar1=1.0 / 128, scalar2=EPS, op0=ALU.mult, op1=ALU.add), reads=["small"], writes=["small2"])

        def emit_norm2(qb, hh):
            fw.op("act", lambda h: h.activation(out=small[:, 16:20], in_=small[:, 12:16], func=AF.Ln), reads=["small2"], writes=["small3"])
            fw.op("act", lambda h: h.activation(out=small[:, 20:24], in_=small[:, 16:20], func=AF.Exp, scale=-0.5), reads=["small3"], writes=["small3"])
            for j in range(4):
                qt = qb * 4 + j
                fw.op("dve", lambda h, qt=qt, j=j: h.scalar_tensor_tensor(out=attn_tok[:, qt, hh * 128:(hh + 1) * 128], in0=osb[:, j, :], scalar=small[:, 20 + j:21 + j], in1=gsub[:], op0=ALU.mult, op1=ALU.mult),
                      reads=["osb", "small3", "gsub"], writes=["attn_tok"])

        emit_S(0)
        pending = None
        for n in range(len(iters)):
            qb, hh, kt = iters[n]
            emit_exp(n)
            if n + 1 < len(iters):
                emit_S(n + 1)
            emit_AV(n)
            if kt == 12 and pending is not None:
                emit_norm2(*pending)
                pending = None
            if kt == NKT - 1:
                emit_norm1(qb, hh)
                pending = (qb, hh)
        emit_norm2(*pending)

        fw.barrier()
        if debug:
            fw.dma("pool", "dbg", lambda h: h.dma_start(out=dbg_attn, in_=attn_tok[:]), reads=["attn_tok"], writes=["dbg_attn"])
        wa2b = [carve(O_POST_C + m * 16384, [8, 1024], BF16) for m in range(4)]
        WKEEP = ["wa2b%d" % m for m in range(4)]
        for m in range(4):
            fw.dma("pool", "cast", lambda h, m=m: h.dma_start(out=wa2b[m], in_=wada_d[:, (m + 2) * D:(m + 3) * D].rearrange("(c p) n -> p c n", p=128)), writes=["wa2b%d" % m])
        o = 0
        ZT = carve(o, [2, 16, 512], BF16); o += 32768
        PTt = carve(o, [2, 4, 2048], BF16); o += 32768
        mixT = carve(o, [8, 2048], BF16); o += 32768
        csF = carve(o, [2, 128], F32); o += 1024
        wfF = carve(o, [4, 128], F32); o += 2048
        O_POST = o
        assert O_POST == O_POST_C, O_POST
        for hh in range(2):
            for ri in range(2):
                r0 = ri * 32 + hh * 16
                fw.dma("sp", "ld", lambda h, hh=hh, ri=ri, r0=r0: h.dma_start(out=ZT[hh * 64:(hh + 1) * 64, ri, :, :], in_=zscr[r0:r0 + 16, :, :].rearrange("i b c -> b i c")), reads=["zscr"], writes=["ZT"])
        ld(csF, cs_d.rearrange("e p n -> p e n"), "csF")
        ld(wfF, wf_d.rearrange("g p n -> p g n"), "wfF")
        for g in range(4):
            for e in range(2):
                fw.op("pe", lambda h, g=g, e=e: h.matmul(PS[(g * 2 + e) // 4][:, 0, ((g * 2 + e) % 4) * 128:((g * 2 + e) % 4) * 128 + 128], lhsT=csF[:, e, :], rhs=wfF[:, g, :], start=True, stop=True),
                      reads=["csF", "wfF"], writes=["pAB"])
        for g in range(4):
            for e in range(2):
                k = g * 2 + e
                fw.op("dve", lambda h, g=g, e=e, k=k: h.tensor_copy(out=AB[:, e, g, :], in_=PS[k // 4][:, 0, (k % 4) * 128:(k % 4) * 128 + 128]), reads=["pAB"], writes=["AB"])
        fw.barrier(keep=WKEEP)
        pairs = [((0, 0), (1, 1)), ((0, 2), (1, 0))]
        for i in range(16):
            pb = i % 2
            for e in range(2):
                for g in range(4):
                    for n, (ri, eb) in enumerate(pairs[e]):
                        fw.op("pe", lambda h, pb=pb, e=e, g=g, ri=ri, eb=eb, n=n, i=i: h.matmul(PS[pb * 2 + e][:, 0, g * 128:(g + 1) * 128], lhsT=ZT[:, ri, i, g * 128:(g + 1) * 128], rhs=EB[:, eb, :], start=(n == 0), stop=(n == 1)),
                              reads=["ZT", "EB"], writes=["pPT%d%d" % (pb, e)])
                eng = "dve" if e == 0 else "act"
                if e == 0:
                    fw.op("dve", lambda h, pb=pb, e=e, i=i: h.tensor_copy(out=PTt[:, e, :, i * 128:(i + 1) * 128], in_=PS[pb * 2 + e][:, 0, :].rearrange("p (g a) -> p g a", g=4)), reads=["pPT%d%d" % (pb, e)], writes=["PTt"])
                else:
                    fw.op("act", lambda h, pb=pb, e=e, i=i: h.copy(out=PTt[:, e, :, i * 128:(i + 1) * 128], in_=PS[pb * 2 + e][:, 0, :].rearrange("p (g a) -> p g a", g=4)), reads=["pPT%d%d" % (pb, e)], writes=["PTt"])
        fw.barrier(keep=WKEEP)
        for g in range(4):
            for tb in range(4):
                k = (g * 4 + tb) % 4
                for e in range(2):
                    fw.op("pe", lambda h, g=g, tb=tb, e=e, k=k: h.matmul(PS[k][:, 0, :], lhsT=AB[:, e, g, :], rhs=PTt[:, e, g, tb * 512:(tb + 1) * 512], start=(e == 0), stop=(e == 1)),
                          reads=["AB", "PTt"], writes=["pR%d" % k])
                if tb % 2 == 0:
                    fw.op("dve", lambda h, g=g, tb=tb, k=k: h.tensor_copy(out=mixT[:, 4 + g, tb * 512:(tb + 1) * 512], in_=PS[k][:, 0, :]), reads=["pR%d" % k], writes=["mixT"])
                else:
                    fw.op("act", lambda h, g=g, tb=tb, k=k: h.copy(out=mixT[:, 4 + g, tb * 512:(tb + 1) * 512], in_=PS[k][:, 0, :]), reads=["pR%d" % k], writes=["mixT"])
        for i in range(16):
            pbk = 1 + 2 * (i % 2)
            pv = bank_bf(pbk).rearrange("p (c n) -> p c n", c=8)
            for hh in range(4):
                fw.op("pe", lambda h, i=i, hh=hh, pv=pv: h.transpose(out=pv[:, hh, :], in_=attn_tok[:, i, hh * 128:(hh + 1) * 128], identity=identB[:]), reads=["attn_tok", "identB"], writes=["pAT%d" % (i % 2)])
            fw.op("act" if i % 2 else "dve", (lambda h, i=i, pv=pv: h.copy(out=mixT[:, 0:4, i * 128:(i + 1) * 128], in_=pv[:, 0:4, :])) if i % 2 else (lambda h, i=i, pv=pv: h.tensor_copy(out=mixT[:, 0:4, i * 128:(i + 1) * 128], in_=pv[:, 0:4, :])),
                  reads=["pAT%d" % (i % 2)], writes=["mixT"])
        fw.barrier(keep=WKEEP)

        if debug:
            fw.dma("pool", "dbg", lambda h: h.dma_start(out=dbg_fo, in_=mixT[:, 4:8, :]), reads=["mixT"], writes=["dbg_fo"])
            fw.barrier(keep=WKEEP)
        o = 32768
        Smat = carve(o, [8, 128], BF16); o += 4096
        btmp = carve(o, [1024], F32); o += 4096
        assert o <= 40960
        bc = {}
        for nm in ("gt1", "sh2", "a2", "gt2", "gfin"):
            bc[nm] = carve(o, [1024], F32); o += 4096
        Lall = carve(o, [NOWN, 36], F32); o += 4096
        assert o <= 65536
        Wout = carve(0, [8, 1024], BF16)
        for c in range(8):
            fw.dma("pool", "cast", lambda h, c=c: h.dma_start(out=Wout[:, c, :], in_=wout_d[c * 128:(c + 1) * 128, :]), writes=["Wout"])
        xlb = [carve(O_POST + 32768 + k * 4096, [1024], F32) for k in range(3)]
        for c in range(8):
            fw.op("dve", lambda h, c=c: h.tensor_scalar(out=Smat[:, c, :], in0=ones_f[:], scalar1=scT[:, c, 0:1], scalar2=0.0, op0=ALU.mult, op1=ALU.add), reads=["ones_f", "scT"], writes=["Smat"])
        ld(bc["gfin"], gfin_d.partition_broadcast(128)[:, 0, :], "bc_gfin")
        ld(bc["a2"], gffn_d.partition_broadcast(128)[:, 0, :], "bc_a2")
        for (mod, nm) in ((2, "gt1"), (3, "sh2"), (4, "sc2"), (5, "gt2")):
            wa2 = wa2b[mod - 2]
            wk = "wa2b%d" % (mod - 2)
            fw.dma("sp", "ld", lambda h, mod=mod: h.dma_start(out=btmp, in_=bada_d[0:1, mod * D:(mod + 1) * D].partition_broadcast(128)[:, 0, :]), writes=["btmp"])
            for half in range(2):
                pm = bank(half)
                for kc in range(8):
                    fw.op("pe", lambda h, kc=kc, half=half, pm=pm, wa2=wa2: h.matmul(pm, lhsT=Smat[:, kc, :], rhs=wa2[:, kc, half * 512:(half + 1) * 512], start=(kc == 0), stop=(kc == 7)), reads=["Smat", wk], writes=["pm%d" % half])
                hs = slice(half * 512, (half + 1) * 512)
                if nm == "sc2":
                    fw.op("dve", lambda h, hs=hs, pm=pm: h.tensor_tensor(out=btmp[:, hs], in0=pm, in1=btmp[:, hs], op=ALU.add), reads=["pm%d" % half, "btmp"], writes=["btmp"])
                    fw.op("dve", lambda h, hs=hs: h.scalar_tensor_tensor(out=bc["a2"][:, hs], in0=btmp[:, hs], scalar=1.0, in1=bc["a2"][:, hs], op0=ALU.add, op1=ALU.mult), reads=["btmp", "bc_a2"], writes=["bc_a2"])
                else:
                    fw.op("dve", lambda h, hs=hs, pm=pm, nm=nm: h.tensor_tensor(out=bc[nm][:, hs], in0=pm, in1=btmp[:, hs], op=ALU.add), reads=["pm%d" % half, "btmp"], writes=["bc_" + nm])
        fw.barrier(keep=["Wout"])

        o = 16384
        h2 = [carve(o + i * 4096, [1024], F32) for i in range(2)]; o += 8192
        tY = [carve(o + i * 4096, [1024], F32) for i in range(2)]; o += 8192
        h2T = carve(o, [8, 128], F32); o += 4096
        junkf = carve(o, [1024], BF16); o += 2048
        stat2 = carve(o, [NOWN, 8], F32); o += NOWN * 32
        assert o <= 40960
        h2ball = carve(O_POST, [NOWN, 1024], BF16)
        RBt = carve(146432, [12, NOWN, 32], F32)

        def opX(i):
            s = i % 2
            xlt = xlb[i % 3]
            xkey = "xlb%d" % (i % 3)
            fw.dma("sp", "ld", lambda h: h.dma_start(out=xlt, in_=x_d[i]), writes=[xkey])
            for half in range(2):
                for c in range(8):
                    fw.op("pe", lambda h, c=c, half=half: h.matmul(bank(half), lhsT=mixT[:, c, i * 128:(i + 1) * 128], rhs=Wout[:, c, half * 512:(half + 1) * 512], start=(c == 0), stop=(c == 7)),
                          reads=["mixT", "Wout"], writes=["pY%d" % half])
                fw.op("dve", lambda h, half=half: h.tensor_tensor(out=tY[s][:, half * 512:(half + 1) * 512], in0=bank(half), in1=bc["gt1"][:, half * 512:(half + 1) * 512], op=ALU.mult), reads=["pY%d" % half, "bc_gt1"], writes=["tY%d" % s])
            fw.op("dve", lambda h: h.tensor_tensor(out=xlt, in0=xlt, in1=tY[s], op=ALU.add), reads=[xkey, "tY%d" % s], writes=[xkey])
            fw.dma("sp", "st", lambda h: h.dma_start(out=xlscr[i], in_=xlt), reads=[xkey], writes=["xlscr"])
            if debug:
                fw.dma("sp", "dbg", lambda h: h.dma_start(out=dbg_xl[i], in_=xlt), reads=[xkey], writes=["dbg_xl"])
            fw.op("act", lambda h: h.activation(out=junkf, in_=xlt, func=AF.Square, accum_out=stat2[:, i, 0:1]), reads=[xkey], writes=["junkf", "st2_%d" % i])
            fw.op("dve", lambda h: h.tensor_scalar(out=stat2[:, i, 1:2], in0=stat2[:, i, 0:1], scalar1=1.0 / D, scalar2=EPS, op0=ALU.mult, op1=ALU.add), reads=["st2_%d" % i], writes=["st2_%d" % i])
            fw.op("act", lambda h: h.activation(out=stat2[:, i, 2:3], in_=stat2[:, i, 1:2], func=AF.Sqrt), reads=["st2_%d" % i], writes=["st2_%d" % i])
            fw.op("dve", lambda h: h.reciprocal(out=stat2[:, i, 3:4], in_=stat2[:, i, 2:3]), reads=["st2_%d" % i], writes=["st2_%d" % i])

        def opY(i):
            s = i % 2
            xlt = xlb[i % 3]
            xkey = "xlb%d" % (i % 3)
            fw.op("dve", lambda h: h.scalar_tensor_tensor(out=h2[s], in0=xlt, scalar=stat2[:, i, 3:4], in1=bc["a2"], op0=ALU.mult, op1=ALU.mult), reads=[xkey, "st2_%d" % i, "bc_a2"], writes=["h2_%d" % s])
            fw.op("dve", lambda h: h.tensor_tensor(out=h2[s], in0=h2[s], in1=bc["sh2"], op=ALU.add), reads=["h2_%d" % s, "bc_sh2"], writes=["h2_%d" % s])
            fw.op("act", lambda h: h.copy(out=h2ball[:, i, :], in_=h2[s]), reads=["h2_%d" % s], writes=["h2b%d" % i])
            for half in range(2):
                pv = PS[1][:, half, :].rearrange("p (c n) -> p c n", c=4)
                for c in range(4):
                    cc = half * 4 + c
                    fw.op("pe", lambda h, cc=cc, c=c, pv=pv: h.transpose(out=pv[:, c, :], in_=h2[s][:, cc * 128:(cc + 1) * 128], identity=identF[:]), reads=["h2_%d" % s, "identF"], writes=["pHT%d" % half])
                if half:
                    fw.op("act", lambda h, pv=pv: h.copy(out=h2T[:, 4:8, :], in_=pv), reads=["pHT1"], writes=["h2T"])
                else:
                    fw.op("dve", lambda h, pv=pv: h.tensor_copy(out=h2T[:, 0:4, :], in_=pv), reads=["pHT0"], writes=["h2T"])
            pl = PS[2][:, 0, 0:36]
            for c in range(8):
                fw.op("pe", lambda h, c=c: h.matmul(pl, lhsT=h2T[:, c, :], rhs=wr[:, c, :], start=(c == 0), stop=False), reads=["h2T", "wr"], writes=["pL"])
            fw.op("pe", lambda h: h.matmul(pl, lhsT=ones_f[0:1, :], rhs=brow[0:1, :], start=False, stop=True), reads=["ones_f", "brow"], writes=["pL"])
            fw.op("dve", lambda h: h.tensor_copy(out=Lall[:, i, :], in_=pl), reads=["pL"], writes=["Lall"])

        opX(0)
        for i in range(NOWN):
            if i + 1 < NOWN:
                opX(i + 1)
            opY(i)

        def RB(k, w=32):
            return RBt[:, k, :, 0:w]

        def bcl(ap2, w):
            return ap2.unsqueeze(2).to_broadcast([128, NOWN, w])

        def V(fn, reads=("rb",), writes=("rb",), eng="dve"):
            fw.op(eng, fn, reads=list(reads), writes=list(writes))

        G = Lall[:, :, 0:4]
        E4 = Lall[:, :, 4:36].rearrange("p i (g j) -> p i g j", g=4)
        c1 = lambda k: RBt[:, 11, :, k]
        V(lambda h: h.tensor_reduce(out=c1(0), in_=G, axis=AX.X, op=ALU.max), reads=("Lall", "rb"))
        V(lambda h: h.tensor_tensor(out=RB(0, 4), in0=G, in1=bcl(c1(0), 4), op=ALU.is_equal), reads=("Lall", "rb"))
        V(lambda h: h.tensor_tensor(out=RB(1, 4), in0=G, in1=bcl(c1(0), 4), op=ALU.subtract), reads=("Lall", "rb"))
        V(lambda h: h.activation(out=RB(2, 4), in_=RB(1, 4), func=AF.Exp), eng="act")
        V(lambda h: h.tensor_reduce(out=c1(1), in_=RB(2, 4), axis=AX.X, op=ALU.add))
        V(lambda h: h.reciprocal(out=c1(2), in_=c1(1)))
        V(lambda h: h.tensor_scalar(out=RB(3, 4), in0=RB(0, 4), scalar1=1e9, scalar2=-1e9, op0=ALU.mult, op1=ALU.add))
        M4 = RB(4).rearrange("p i (g j) -> p i g j", g=4)
        V(lambda h: h.tensor_tensor(out=M4, in0=E4, in1=RB(3, 4).unsqueeze(3).to_broadcast([128, NOWN, 4, 8]), op=ALU.add), reads=("Lall", "rb"))
        V(lambda h: h.tensor_reduce(out=c1(3), in_=RB(4), axis=AX.X, op=ALU.max))
        V(lambda h: h.tensor_tensor(out=RB(5), in0=RB(4), in1=bcl(c1(3), 32), op=ALU.is_equal))
        V(lambda h: h.scalar_tensor_tensor(out=RB(6), in0=RB(5), scalar=-1e9, in1=RB(4), op0=ALU.mult, op1=ALU.add))
        V(lambda h: h.tensor_reduce(out=c1(4), in_=RB(6), axis=AX.X, op=ALU.max))
        V(lambda h: h.tensor_tensor(out=RB(7), in0=RB(6), in1=bcl(c1(4), 32), op=ALU.is_equal))
        V(lambda h: h.tensor_tensor(out=c1(5), in0=c1(4), in1=c1(3), op=ALU.subtract))
        V(lambda h: h.activation(out=c1(6), in_=c1(5), func=AF.Exp), eng="act")
        V(lambda h: h.tensor_scalar(out=c1(7), in0=c1(6), scalar1=1.0, scalar2=0.0, op0=ALU.add, op1=ALU.add))
        V(lambda h: h.reciprocal(out=c1(8), in_=c1(7)))
        V(lambda h: h.tensor_tensor(out=gates[:, :, 0], in0=c1(8), in1=c1(2), op=ALU.mult), writes=("rb", "gates"))
        V(lambda h: h.tensor_tensor(out=gates[:, :, 1], in0=gates[:, :, 0], in1=c1(6), op=ALU.mult), reads=("rb", "gates"), writes=("rb", "gates"))
        V(lambda h: h.tensor_tensor(out=Bt[:], in0=RB(5), in1=RB(7), op=ALU.add), writes=("rb", "Bt"))
        pp = PS[2][:, 1, :].rearrange("p (i e) -> p i e", i=NOWN)
        for i in range(NOWN):
            for i2 in range(i + 1):
                fw.op("pe", lambda h, i2=i2, i=i: h.matmul(pp[:, i, :], lhsT=(lstB[:] if i2 == i else ones_b[:]), rhs=Bt[:, i2, :], start=(i2 == 0), stop=(i2 == i), skip_group_check=True), reads=["Bt", "lstB", "ones_b"], writes=["pP"])
        V(lambda h: h.tensor_copy(out=RB(8), in_=pp), reads=("pP", "rb"))
        iob = iotaF[:].unsqueeze(1).to_broadcast([128, NOWN, 32])
        for k, ohk in ((0, 5), (1, 7)):
            V(lambda h, ohk=ohk: h.tensor_tensor(out=RB(9), in0=RB(ohk), in1=RB(8), op=ALU.mult))
            V(lambda h: h.tensor_reduce(out=c1(9), in_=RB(9), axis=AX.X, op=ALU.add))
            V(lambda h, ohk=ohk: h.tensor_tensor(out=RB(9), in0=RB(ohk), in1=iob, op=ALU.mult), reads=("rb", "iotaF"))
            V(lambda h: h.tensor_reduce(out=c1(10), in_=RB(9), axis=AX.X, op=ALU.add))
            V(lambda h: h.tensor_scalar(out=c1(11), in0=c1(9), scalar1=float(CAP), scalar2=1e6, op0=ALU.is_ge, op1=ALU.mult))
            V(lambda h: h.scalar_tensor_tensor(out=c1(12), in0=c1(10), scalar=float(CAP), in1=c1(9), op0=ALU.mult, op1=ALU.add))
            V(lambda h: h.tensor_tensor(out=c1(13), in0=c1(12), in1=c1(11), op=ALU.add))
            V(lambda h: h.tensor_scalar(out=c1(14), in0=c1(13), scalar1=float(NSLOT), scalar2=0.0, op0=ALU.min, op1=ALU.add))
            V(lambda h, k=k: h.tensor_copy(out=desti[:, :, k, 0], in_=c1(13)), writes=("rb", "desti"))
            V(lambda h, k=k: h.tensor_copy(out=desti[:, :, k, 1], in_=c1(14)), writes=("rb", "desti"))
        NW = 3
        wbase = [0, 61440, 61440 + 24576]
        Wg = [carve(wbase[k], [8, 512], BF16) for k in range(NW)]
        Wu = [carve(wbase[k] + 8192, [8, 512], BF16) for k in range(NW)]
        Wd = [carve(wbase[k] + 16384, [4, 1024], BF16) for k in range(NW)]
        def load_w(e, which, extra=()):
            ws = e % NW
            if which == 0:
                fw.dma("pool", "wcast", lambda h: h.dma_start(out=Wg[ws].rearrange("p (a b) f -> p a (b f)", a=2), in_=wg_d[e].rearrange("(p a b) f -> p a (b f)", a=2, b=4)), writes=["Wg%d" % ws] + list(extra))
            elif which == 1:
                fw.dma("pool", "wcast", lambda h: h.dma_start(out=Wu[ws].rearrange("p (a b) f -> p a (b f)", a=2), in_=wu_d[e].rearrange("(p a b) f -> p a (b f)", a=2, b=4)), writes=["Wu%d" % ws] + list(extra))
            else:
                fw.dma("pool", "wcast", lambda h: h.dma_start(out=Wd[ws].rearrange("p (a b) f -> p a (b f)", a=2), in_=wd_d[e].rearrange("(p a b) f -> p a (b f)", a=2, b=2)), writes=["Wd%d" % ws] + list(extra))

        for w in range(3):
            load_w(0, w, extra=["Wout", "h2_0", "h2_1"])
        for w in range(3):
            load_w(1, w, extra=["Lall", "mixT"])
        for i in range(NOWN):
            for k in range(2):
                fw.dma("pool", "ind", lambda h, i=i, k=k: h.indirect_dma_start(out=xslots, out_offset=bass.IndirectOffsetOnAxis(ap=desti[:, i, k, 0:1], axis=0), in_=h2ball[:, i, :], in_offset=None, bounds_check=NSLOT - 1, oob_is_err=False),
                       reads=["h2b%d" % i, "desti"], writes=["xslots"])
        if debug:
            fw.dma("sp", "dbg", lambda h: h.dma_start(out=dbg_gates, in_=gates[:]), reads=["gates"], writes=["dbg_gates"])
            fw.dma("sp", "dbg", lambda h: h.dma_start(out=dbg_dest, in_=desti[:]), reads=["desti"], writes=["dbg_dest"])
        fw.barrier(keep=["Wg0", "Wu0", "Wd0", "Wg1", "Wu1", "Wd1"])

        o = 24576
        xbT = [carve(o + i * 2048, [8, 128], BF16) for i in range(2)]; o += 4096
        sg = [carve(o + i * 2048, [512], F32) for i in range(2)]; o += 4096
        hm = [carve(o + i * 1024, [512], BF16) for i in range(2)]; o += 2048
        hmT = [carve(o + i * 1024, [4, 128], BF16) for i in range(2)]; o += 2048
        assert o <= 40960, o
        o = 61440 + 2 * 24576
        NXB, PF = 6, 4
        YG0 = o
        xb = [carve(o + i * 2048, [1024], BF16) for i in range(NXB)]; o += NXB * 2048
        NYS = 4
        ysb = [carve(o + i * 4096, [1024], F32) for i in range(NYS)]; o += NYS * 4096
        y01 = [carve(o + i * 4096, [1024], F32) for i in range(2)]; o += 8192
        xlf = [carve(o + i * 4096, [1024], F32) for i in range(2)]; o += 8192
        assert o <= USZ, o
        fw.op("dve", lambda h: h.memset(ysb[1][0:1, :], 0.0), writes=["ysb1"])
        fw.dma("sp", "st", lambda h: h.dma_start(out=ys[NSLOT:NSLOT + 1, :], in_=ysb[1][0:1, :]), reads=["ysb1"], writes=["ys"])
        NBLK = CAP // 128
        NB = 32 * NBLK

        def load_xb(blk):
            e, b_ = blk // NBLK, blk % NBLK
            r0 = e * CAP + b_ * 128
            k = blk % NXB
            fw.dma("sp", "ld", lambda h: h.dma_start(out=xb[k], in_=xslots[r0:r0 + 128, :]), reads=["xslots"], writes=["xb%d" % k])

        def mA(blk):
            e, b_ = blk // NBLK, blk % NBLK
            ws = e % NW
            s_ = blk % 2
            k = blk % NXB
            if e + 2 < 32 and b_ < 3:
                load_w(e + 2, b_)
            if blk + PF < NB:
                load_xb(blk + PF)
            pT = bank_bf(4).rearrange("p (c n) -> p c n", c=8)
            for c in range(8):
                fw.op("pe", lambda h, c=c: h.transpose(out=pT[:, c, :], in_=xb[k].rearrange("p (n c) -> p c n", c=8)[:, c, :], identity=identB[:]), reads=["xb%d" % k, "identB"], writes=["b4"])
            fw.op("dve", lambda h: h.tensor_copy(out=xbT[s_], in_=pT), reads=["b4"], writes=["xbT%d" % s_])
            for c in range(8):
                fw.op("pe", lambda h, c=c: h.matmul(bank(2 * s_), lhsT=xbT[s_][:, c, :], rhs=Wg[ws][:, c, :], start=(c == 0), stop=(c == 7)), reads=["xbT%d" % s_, "Wg%d" % ws], writes=["pG%d" % s_])
            for c in range(8):
                fw.op("pe", lambda h, c=c: h.matmul(bank(2 * s_ + 1), lhsT=xbT[s_][:, c, :], rhs=Wu[ws][:, c, :], start=(c == 0), stop=(c == 7)), reads=["xbT%d" % s_, "Wu%d" % ws], writes=["pU%d" % s_])

        def mB(blk):
            e, b_ = blk // NBLK, blk % NBLK
            ws = e % NW
            s_ = blk % 2
            ky = blk % NYS
            r0 = e * CAP + b_ * 128
            fw.op("act", lambda h: h.activation(out=sg[s_], in_=bank(2 * s_), func=AF.Silu), reads=["pG%d" % s_], writes=["sg%d" % s_])
            fw.op("dve", lambda h: h.tensor_tensor(out=hm[s_], in0=sg[s_], in1=bank(2 * s_ + 1), op=ALU.mult), reads=["sg%d" % s_, "pU%d" % s_], writes=["hm%d" % s_])
            pH = bank_bf(5).rearrange("p (c n) -> p c n", c=8)
            for c in range(4):
                fw.op("pe", lambda h, c=c: h.transpose(out=pH[:, c, :], in_=hm[s_].rearrange("p (n c) -> p c n", c=4)[:, c, :], identity=identB[:]), reads=["hm%d" % s_, "identB"], writes=["b5"])
            fw.op("act", lambda h: h.copy(out=hmT[s_], in_=pH[:, 0:4, :]), reads=["b5"], writes=["hmT%d" % s_])
            for half in range(2):
                for c in range(4):
                    fw.op("pe", lambda h, c=c, half=half: h.matmul(PS[3][:, half, :], lhsT=hmT[s_][:, c, :], rhs=Wd[ws][:, c, half * 512:(half + 1) * 512], start=(c == 0), stop=(c == 3)), reads=["hmT%d" % s_, "Wd%d" % ws], writes=["pD%d" % half])
            fw.op("dve", lambda h: h.tensor_copy(out=ysb[ky][:, 0:512], in_=PS[3][:, 0, :]), reads=["pD0"], writes=["ysb%d" % ky])
            fw.op("act", lambda h: h.copy(out=ysb[ky][:, 512:1024], in_=PS[3][:, 1, :]), reads=["pD1"], writes=["ysb%d" % ky])
            fw.dma("sp", "st", lambda h: h.dma_start(out=ys[r0:r0 + 128, :], in_=ysb[ky]), reads=["ysb%d" % ky], writes=["ys"])

        for blk in range(PF):
            load_xb(blk)
        for step in range(NB + 1):
            if step < NB:
                mA(step)
            if step >= 1:
                mB(step - 1)
        fw.barrier()

        outs = []
        NY = 3
        yg = [[carve(YG0 + (r * 2 + k) * 4096, [1024], F32) for k in range(2)] for r in range(NY)]
        xlf3 = [carve(YG0 + NY * 8192 + r * 4096, [1024], F32) for r in range(3)]

        def fin_load(i):
            r = i % NY
            for k in range(2):
                fw.dma("pool", "ind", lambda h, k=k: h.indirect_dma_start(out=yg[r][k], out_offset=None, in_=ys, in_offset=bass.IndirectOffsetOnAxis(ap=desti[:, i, k, 1:2], axis=0)),
                       reads=["ys", "desti"], writes=["yg%d_%d" % (r, k)])
            fw.dma("sp", "ld", lambda h: h.dma_start(out=xlf3[i % 3], in_=xlscr[i]), reads=["xlscr"], writes=["xlf%d" % (i % 3)])

        def fin_comp(i):
            r = i % NY
            y0, y1 = yg[r]
            k0, k1 = "yg%d_0" % r, "yg%d_1" % r
            xf = xlf3[i % 3]
            xfk = "xlf%d" % (i % 3)
            fw.op("dve", lambda h: h.tensor_scalar(out=y0, in0=y0, scalar1=gates[:, i, 0:1], scalar2=0.0, op0=ALU.mult, op1=ALU.add), reads=[k0, "gates"], writes=[k0])
            fw.op("dve", lambda h: h.scalar_tensor_tensor(out=y0, in0=y1, scalar=gates[:, i, 1:2], in1=y0, op0=ALU.mult, op1=ALU.add), reads=[k0, k1, "gates"], writes=[k0])
            fw.op("dve", lambda h: h.tensor_tensor(out=y0, in0=y0, in1=bc["gt2"], op=ALU.mult), reads=[k0, "bc_gt2"], writes=[k0])
            fw.op("dve", lambda h: h.tensor_tensor(out=xf, in0=xf, in1=y0, op=ALU.add), reads=[k0, xfk], writes=[xfk])
            fw.op("act", lambda h: h.activation(out=junkf, in_=xf, func=AF.Square, accum_out=stat2[:, i, 4:5]), reads=[xfk], writes=["junkf", "st3_%d" % i])
            fw.op("dve", lambda h: h.tensor_scalar(out=stat2[:, i, 5:6], in0=stat2[:, i, 4:5], scalar1=1.0 / D, scalar2=EPS, op0=ALU.mult, op1=ALU.add), reads=["st3_%d" % i], writes=["st3_%d" % i])
            fw.op("act", lambda h: h.activation(out=stat2[:, i, 6:7], in_=stat2[:, i, 5:6], func=AF.Sqrt), reads=["st3_%d" % i], writes=["st3_%d" % i])
            fw.op("dve", lambda h: h.reciprocal(out=stat2[:, i, 7:8], in_=stat2[:, i, 6:7]), reads=["st3_%d" % i], writes=["st3_%d" % i])
            fw.op("act", lambda h: h.activation(out=xf, in_=xf, func=AF.Copy, scale=stat2[:, i, 7:8]), reads=[xfk, "st3_%d" % i], writes=[xfk])
            fw.op("dve", lambda h: h.tensor_tensor(out=xf, in0=xf, in1=bc["gfin"], op=ALU.mult), reads=[xfk, "bc_gfin"], writes=[xfk])
            fw.dma("sp", "out", lambda h: h.dma_start(out=y_d[i], in_=xf), reads=[xfk], writes=["y%d" % i])
            outs.append("y%d" % i)

        fin_load(0)
        fin_load(1)
        for i in range(NOWN):
            if i + 2 < NOWN:
                fin_load(i + 2)
            fin_comp(i)
        fw.final_wait("sp", outs)
        fw.emit(st)
    return nc


def _consts(j):
    f32 = np.float32
    bs = (16 * j + np.arange(NT)) % NT
    a = np.arange(128)
    inv_freq = 10000.0 ** (-np.arange(0, 32, 2, dtype=np.float64) / 32)
    rope = np.zeros((NT, 128, 128), f32)
    for u in range(NT):
        ang_r = a[:, None] * inv_freq[None, :]
        ang_c = np.full((128, 1), float(bs[u])) * inv_freq[None, :]
        ang = np.concatenate([ang_r, ang_r, ang_c, ang_c], axis=1)
        sgn = np.concatenate([-np.ones(16), np.ones(16), -np.ones(16), np.ones(16)])
        rope[u, :, 0:64] = np.cos(ang.astype(f32))
        rope[u, :, 64:128] = np.sin(ang.astype(f32)) * sgn
    cset = np.array([16 * j + i + 64 * h for h in range(2) for i in range(16)])
    n = 64 * a[:, None] + bs[None, :]
    th = 2 * np.pi * ((n[:, :, None] * cset[None, None, :]) % 8192) / 8192.0
    mb = np.concatenate([np.cos(th), -np.sin(th)], axis=2) / 32.0
    eb = np.zeros((3, 128, 128))
    d = np.arange(64)
    for h in range(2):
        for u in range(NT):
            ph = 2 * np.pi * ((bs[u] * d) % 64) / 64.0
            eb[0, h * 64 + u, 2 * d + h] = np.cos(ph) / 32.0
            eb[1, h * 64 + u, 2 * d + h] = np.sin(ph) / 32.0
    eb[2] = -eb[1]
    cc = np.arange(128)
    phc = 2 * np.pi * ((cc[:, None] * cc[None, :]) % 128) / 128.0
    cs = np.stack([np.cos(phc), np.sin(phc)])
    lst = (a[:, None] < a[None, :]).astype(f32)
    iota = np.tile(np.arange(32, dtype=f32)[None, :], (128, 1))
    return dict(rope=rope, mb=mb.astype(f32), eb=eb.astype(f32), cs=cs.astype(f32), lstrict=lst, iota=iota,
                ident=np.eye(128, dtype=f32))


_NC = None


def kernel(x, c, ctx, c_ctx, w_ada, b_ada, g_mix_norm, g_ffn_norm, w_in,
           lambda_q1, lambda_k1, lambda_q2, lambda_k2, g_subln, w_fourier, w_out,
           w_router_group, b_router_group, w_router_expert, b_router_expert,
           w_gate, w_up, w_down, g_final):
    global _NC
    if _NC is None:
        _NC = build_nc()
    nc = _NC
    in_maps = make_inputs(x, c, ctx, c_ctx, w_ada, b_ada, g_mix_norm, g_ffn_norm, w_in,
                          lambda_q1, lambda_k1, lambda_q2, lambda_k2, g_subln, w_fourier, w_out,
                          w_router_group, b_router_group, w_router_expert, b_router_expert,
                          w_gate, w_up, w_down, g_final)
    res = run_bass_kernel_spmd(nc, in_maps, core_ids=list(range(8)))
    f32 = np.float32
    out = np.zeros((2, 128, NT, D), f32)
    for core in range(8):
        b, j = core // 4, core % 4
        y = res.results[core]["y"]
        out[b][:, 16 * j:16 * j + 16, :] = y.transpose(1, 0, 2)
    return out.reshape(2, 8192, D)


def make_inputs(x, c, ctx, c_ctx, w_ada, b_ada, g_mix_norm, g_ffn_norm, w_in,
                lambda_q1, lambda_k1, lambda_q2, lambda_k2, g_subln, w_fourier, w_out,
                w_router_group, b_router_group, w_router_expert, b_router_expert,
                w_gate, w_up, w_down, g_final):
    f32 = np.float32
    A = lambda v: np.ascontiguousarray(np.asarray(v, dtype=f32))
    x = A(x); c = A(c); ctx = A(ctx); c_ctx = A(c_ctx)
    shared = dict(
        w_ada=A(w_ada[0]), b_ada=A(b_ada[0]).reshape(1, -1),
        badaT=A(np.asarray(b_ada[0]).reshape(6, 8, 128).transpose(2, 0, 1)),
        gmixT=A(np.asarray(g_mix_norm[0]).reshape(8, 128).T),
        g_ffn=A(g_ffn_norm[0]).reshape(1, -1), g_final=A(g_final).reshape(1, -1),
        w_in=A(w_in[0]),
        lam4=A(np.concatenate([np.asarray(lambda_q1[0]), np.asarray(lambda_k1[0]), np.asarray(lambda_q2[0]), np.asarray(lambda_k2[0])])).reshape(1, 256),
        g_subln=A(g_subln[0]).reshape(1, 128), w_fourier=A(w_fourier[0]), w_out=A(w_out[0]),
        w_router=A(np.concatenate([np.asarray(w_router_group[0]), np.asarray(w_router_expert[0])], axis=1)),
        b_router=A(np.concatenate([np.asarray(b_router_group[0]), np.asarray(b_router_expert[0])])).reshape(1, 36),
        w_gate=A(w_gate[0]), w_up=A(w_up[0]), w_down=A(w_down[0]),
    )
    consts = [_consts(j) for j in range(4)]
    in_maps = []
    for core in range(8):
        b, j = core // 4, core % 4
        bs = (16 * j + np.arange(NT)) % NT
        xt = x[b].reshape(128, NT, D).transpose(1, 0, 2)[bs]
        cc = np.stack([c[b], c_ctx], axis=1)
        m = dict(shared)
        m.update(consts[j])
        m["x"] = np.ascontiguousarray(xt)
        m["ctx"] = np.ascontiguousarray(ctx[b].reshape(2, 128, D))
        m["ccT"] = np.ascontiguousarray(cc.reshape(8, 128, 2).transpose(1, 0, 2))
        in_maps.append(m)
    return in_maps
```
